# Optimizing a Trainium2 kernel written in Bass

```python
import math
import jax, jax.numpy as jnp
from jax import lax
import numpy as np

D_MODEL = 2048
BATCH = 2
SEQ = 16384
DEPTH = 1

MIX_WIDTH = D_MODEL
ATT_WIDTH = MIX_WIDTH // 2
SSM_WIDTH = MIX_WIDTH - ATT_WIDTH

ATT_HEAD_DIM = 64
ATT_HEADS = ATT_WIDTH // (2 * ATT_HEAD_DIM)
ATT_V_DIM = 2 * ATT_HEAD_DIM
ATT_BLOCK = 128
REL_BUCKETS = 32
REL_MAX_DIST = 128

SSM_HEAD_DIM = 64
SSM_HEADS = SSM_WIDTH // SSM_HEAD_DIM
SSM_GROUPS = 2
SSM_HEADS_PER_GROUP = SSM_HEADS // SSM_GROUPS
SSM_STATE = 128
SSM_CONV = 4
SSM_CHUNK = 128
SSM_CONV_DIM = SSM_WIDTH + 2 * SSM_GROUPS * SSM_STATE

Q_COLS = ATT_HEADS * 2 * ATT_HEAD_DIM
K_COLS = Q_COLS
V_COLS = ATT_HEADS * ATT_V_DIM
Z_COLS = SSM_WIDTH
XBC_COLS = SSM_CONV_DIM
DT_COLS = SSM_HEADS
IN_COLS = Q_COLS + K_COLS + V_COLS + Z_COLS + XBC_COLS + DT_COLS
IN_SPLITS = [Q_COLS, Q_COLS + K_COLS, Q_COLS + K_COLS + V_COLS,
             Q_COLS + K_COLS + V_COLS + Z_COLS,
             Q_COLS + K_COLS + V_COLS + Z_COLS + XBC_COLS]

PEER_HEADS = 8
PEER_NKEYS = 128
PEER_EXPERTS = PEER_NKEYS ** 2
PEER_TOPK = 16
PEER_QDIM = 256
PEER_HALF = PEER_QDIM // 2
PEER_BLOCK = 128

NORM_EPS = 1e-6

kernel_name = "hybrid_diffattn_ssd_peer_layer"


def rms_norm(x, w):
    xf = x.astype(jnp.float32)
    y = xf * lax.rsqrt(jnp.mean(xf * xf, axis=-1, keepdims=True) + NORM_EPS)
    return (y * w.astype(jnp.float32)).astype(x.dtype)


def lambda_init_fn(layer_idx):
    return 0.8 - 0.6 * math.exp(-0.3 * layer_idx)


def t5_bucket(rel):
    n = jnp.maximum(rel, 0)
    max_exact = REL_BUCKETS // 2
    nf = jnp.maximum(n, 1).astype(jnp.float32)
    large = max_exact + (jnp.log(nf / max_exact) / math.log(REL_MAX_DIST / max_exact)
                         * (REL_BUCKETS - max_exact)).astype(jnp.int32)
    large = jnp.minimum(large, REL_BUCKETS - 1)
    return jnp.where(n < max_exact, n, large)


def diff_attention(q, k, v, lam, rel_bias):
    b, s, h, _, dh = q.shape
    nb = s // ATT_BLOCK
    scale = dh ** -0.5
    kpos = jnp.arange(s, dtype=jnp.int32)

    def block(i):
        q_blk = lax.dynamic_slice_in_dim(q, i * ATT_BLOCK, ATT_BLOCK, axis=1)
        logits = jnp.einsum('bqhmd,bkhmd->bhmqk', q_blk, k).astype(jnp.float32) * scale
        qpos = i * ATT_BLOCK + jnp.arange(ATT_BLOCK, dtype=jnp.int32)
        rel = qpos[:, None] - kpos[None, :]
        bias = jnp.transpose(rel_bias[t5_bucket(rel)], (2, 3, 0, 1))
        logits = jnp.where(rel >= 0, logits + bias.astype(jnp.float32), -jnp.inf)
        p = jax.nn.softmax(logits, axis=-1)
        p = p[:, :, 0] - lam * p[:, :, 1]
        return jnp.einsum('bhqk,bkhd->bqhd', p.astype(v.dtype), v)

    out = lax.map(block, jnp.arange(nb, dtype=jnp.int32))
    return jnp.moveaxis(out, 0, 1).reshape(b, s, h, -1)


def causal_depthwise_conv(u, w, bias):
    out = lax.conv_general_dilated(
        u, w[:, None, :].astype(u.dtype), window_strides=(1,), padding=[(SSM_CONV - 1, 0)],
        dimension_numbers=('NWC', 'WIO', 'NWC'), feature_group_count=u.shape[-1])
    return out + bias


def ssd_mixer(z, xbc, dt, conv_w, conv_b, dt_bias, a_log, d_skip, norm_w):
    f32 = jnp.float32
    b, s, _ = z.shape
    G, J, P, N, L = SSM_GROUPS, SSM_HEADS_PER_GROUP, SSM_HEAD_DIM, SSM_STATE, SSM_CHUNK
    nc = s // L
    xbc = jax.nn.silu(causal_depthwise_conv(xbc, conv_w, conv_b)).astype(f32)
    xs, bm, cm = jnp.split(xbc, [SSM_WIDTH, SSM_WIDTH + G * N], axis=-1)
    xs = xs.reshape(b, s, G, J, P)
    dt = jax.nn.softplus(dt.astype(f32) + dt_bias.astype(f32)).reshape(b, s, G, J)
    a = -jnp.exp(a_log.astype(f32)).reshape(G, J)

    xc = (xs * dt[..., None]).reshape(b, nc, L, G, J, P)
    bc = bm.reshape(b, nc, L, G, N)
    cc = cm.reshape(b, nc, L, G, N)
    a_cs = jnp.cumsum((dt * a).reshape(b, nc, L, G, J), axis=2)

    tril = jnp.tril(jnp.ones((L, L), dtype=bool))[:, :, None, None]
    seg = a_cs[:, :, :, None] - a_cs[:, :, None, :]
    decay = jnp.exp(jnp.where(tril, seg, -jnp.inf))
    cb = jnp.einsum('bclgn,bcsgn->bclsg', cc, bc)
    y_diag = jnp.einsum('bclsgj,bcsgjp->bclgjp', cb[..., None] * decay, xc)

    decay_states = jnp.exp(a_cs[:, :, -1:] - a_cs)
    states = jnp.einsum('bclgn,bclgjp->bcgjpn', bc, xc * decay_states[..., None])
    chunk_decay = jnp.exp(a_cs[:, :, -1])

    def step(state, inp):
        s_c, d_c = inp
        return state * d_c[..., None, None] + s_c, state

    init = jnp.zeros((b, G, J, P, N), f32)
    _, prev = lax.scan(step, init, (jnp.moveaxis(states, 1, 0), jnp.moveaxis(chunk_decay, 1, 0)))
    prev = jnp.moveaxis(prev, 0, 1)
    y_off = jnp.einsum('bclgn,bcgjpn->bclgjp', cc, prev) * jnp.exp(a_cs)[..., None]

    y = (y_diag + y_off).reshape(b, s, G, J, P) + d_skip.astype(f32).reshape(G, J)[:, :, None] * xs
    y = y.reshape(b, s, G, J * P) * jax.nn.silu(z.astype(f32)).reshape(b, s, G, J * P)
    y = y * lax.rsqrt(jnp.mean(y * y, axis=-1, keepdims=True) + NORM_EPS)
    y = y.reshape(b, s, SSM_WIDTH) * norm_w.astype(f32)
    return y.astype(z.dtype)


def peer_ffn(u, wq, sub_keys, down, up):
    b, s, d = u.shape
    tokens = u.reshape((b * s) // PEER_BLOCK, PEER_BLOCK, d)

    def block(xb):
        q = (xb @ wq).reshape(PEER_BLOCK, PEER_HEADS, 2, PEER_HALF)
        scores = jnp.einsum('thcd,hckd->thck', q, sub_keys).astype(jnp.float32)
        s1, i1 = lax.top_k(scores[:, :, 0], PEER_TOPK)
        s2, i2 = lax.top_k(scores[:, :, 1], PEER_TOPK)
        cand = (s1[..., :, None] + s2[..., None, :]).reshape(PEER_BLOCK, PEER_HEADS, PEER_TOPK ** 2)
        cand_idx = (i1[..., :, None] * PEER_NKEYS + i2[..., None, :]).reshape(
            PEER_BLOCK, PEER_HEADS, PEER_TOPK ** 2)
        top, pos = lax.top_k(cand, PEER_TOPK)
        idx = jnp.take_along_axis(cand_idx, pos, axis=-1)
        gate = jax.nn.softmax(top, axis=-1)
        pre = jnp.einsum('thkd,td->thk', down[idx], xb).astype(jnp.float32)
        act = jax.nn.gelu(pre, approximate=False) * gate
        return jnp.einsum('thk,thkd->td', act.astype(up.dtype), up[idx])

    return lax.map(block, tokens).reshape(b, s, d)


def setup_inputs(seed: int = 0) -> dict:
    key = jax.random.key(seed)
    ks = jax.random.split(key, 32)
    f32 = jnp.float32
    D = D_MODEL
    nrm = lambda k, shp, sc: jax.random.normal(k, shp, f32) * sc
    dt0 = jnp.exp(jax.random.uniform(ks[15], (DEPTH, SSM_HEADS), f32,
                                     math.log(1e-3), math.log(1e-1)))
    return {
        'x': nrm(ks[0], (BATCH, SEQ, D), 1.0),
        'c': nrm(ks[1], (BATCH, D), 1.0),
        'ada_w': nrm(ks[2], (DEPTH, D, 6 * D), D ** -0.5),
        'ada_b': nrm(ks[3], (DEPTH, 6 * D), 0.01),
        'norm1_w': 1.0 + nrm(ks[4], (DEPTH, D), 0.02),
        'w_in': nrm(ks[5], (DEPTH, D, IN_COLS), D ** -0.5),
        'q_norm_w': 1.0 + nrm(ks[6], (DEPTH, ATT_HEAD_DIM), 0.02),
        'k_norm_w': 1.0 + nrm(ks[7], (DEPTH, ATT_HEAD_DIM), 0.02),
        'rel_bias': nrm(ks[8], (REL_BUCKETS, ATT_HEADS, 2), 0.5),
        'lambda_q1': nrm(ks[9], (DEPTH, ATT_HEAD_DIM), 0.1),
        'lambda_k1': nrm(ks[10], (DEPTH, ATT_HEAD_DIM), 0.1),
        'lambda_q2': nrm(ks[11], (DEPTH, ATT_HEAD_DIM), 0.1),
        'lambda_k2': nrm(ks[12], (DEPTH, ATT_HEAD_DIM), 0.1),
        'subln_w': 1.0 + nrm(ks[13], (DEPTH, ATT_V_DIM), 0.02),
        'conv_w': nrm(ks[14], (DEPTH, SSM_CONV, SSM_CONV_DIM), SSM_CONV ** -0.5),
        'conv_b': nrm(ks[16], (DEPTH, SSM_CONV_DIM), 0.01),
        'dt_bias': dt0 + jnp.log(-jnp.expm1(-dt0)),
        'a_log': jnp.log(jax.random.uniform(ks[17], (DEPTH, SSM_HEADS), f32, 1.0, 16.0)),
        'd_skip': 1.0 + nrm(ks[18], (DEPTH, SSM_HEADS), 0.02),
        'ssm_norm_w': 1.0 + nrm(ks[19], (DEPTH, SSM_WIDTH), 0.02),
        'w_out': nrm(ks[20], (DEPTH, MIX_WIDTH, D), MIX_WIDTH ** -0.5),
        'norm2_w': 1.0 + nrm(ks[21], (DEPTH, D), 0.02),
        'peer_wq': nrm(ks[22], (DEPTH, D, PEER_HEADS * PEER_QDIM), D ** -0.5),
        'peer_keys': nrm(ks[23], (DEPTH, PEER_HEADS, 2, PEER_NKEYS, PEER_HALF), PEER_HALF ** -0.5),
        'expert_down': nrm(ks[24], (DEPTH, PEER_EXPERTS, D), D ** -0.5),
        'expert_up': nrm(ks[25], (DEPTH, PEER_EXPERTS, D), PEER_HEADS ** -0.5),
    }


def reference(x, c, ada_w, ada_b, norm1_w, w_in, q_norm_w, k_norm_w, rel_bias,
              lambda_q1, lambda_k1, lambda_q2, lambda_k2, subln_w, conv_w, conv_b,
              dt_bias, a_log, d_skip, ssm_norm_w, w_out, norm2_w, peer_wq, peer_keys,
              expert_down, expert_up):
    b, s, _ = x.shape
    f32 = jnp.float32
    h = x
    for l in range(DEPTH):
        lam_init = lambda_init_fn(l)
        mod = jax.nn.silu(c) @ ada_w[l] + ada_b[l]
        sh1, sc1, g1, sh2, sc2, g2 = jnp.split(mod[:, None, :], 6, axis=-1)

        hn = rms_norm(h, norm1_w[l]) * (1.0 + sc1) + sh1
        proj = hn @ w_in[l]
        q, k, v, z, xbc, dt = jnp.split(proj, IN_SPLITS, axis=-1)

        q = rms_norm(q.reshape(b, s, ATT_HEADS, 2, ATT_HEAD_DIM), q_norm_w[l])
        k = rms_norm(k.reshape(b, s, ATT_HEADS, 2, ATT_HEAD_DIM), k_norm_w[l])
        v = v.reshape(b, s, ATT_HEADS, ATT_V_DIM)
        lam = (jnp.exp(jnp.sum(lambda_q1[l].astype(f32) * lambda_k1[l].astype(f32)))
               - jnp.exp(jnp.sum(lambda_q2[l].astype(f32) * lambda_k2[l].astype(f32)))
               + lam_init)
        att = diff_attention(q, k, v, lam, rel_bias)
        att = (rms_norm(att, subln_w[l]) * (1.0 - lam_init)).reshape(b, s, ATT_WIDTH)

        ssm = ssd_mixer(z, xbc, dt, conv_w[l], conv_b[l], dt_bias[l], a_log[l],
                        d_skip[l], ssm_norm_w[l])

        mix = jnp.concatenate([att, ssm.astype(att.dtype)], axis=-1) @ w_out[l]
        h = h + g1 * mix

        hn2 = rms_norm(h, norm2_w[l]) * (1.0 + sc2) + sh2
        h = h + g2 * peer_ffn(hn2, peer_wq[l], peer_keys[l], expert_down[l], expert_up[l])
    return h
```

```python
import contextlib, math
import numpy as np
import concourse.bass as bass
import concourse.mybir as mybir
from concourse.bass_utils import run_bass_kernel_spmd

F32 = mybir.dt.float32; BF16 = mybir.dt.bfloat16
AF = mybir.ActivationFunctionType; ALU = mybir.AluOpType; AX = mybir.AxisListType

D = 2048; NCH = 16; H = 8; NEXP = 16384
EPS = 1e-6
LAM_INIT = 0.2
SAME_ENGINE_SYNC = True
ATTACH_WAIT = True

class _Eng:
    def __init__(self, name, eng, sem):
        self.name = name; self.eng = eng; self.sem = sem; self.count = 0; self.seen = {}

class _Stream:
    def __init__(self, sem):
        self.sem = sem; self.count = 0

class Ctx:
    def __init__(self, nc, es):
        self.nc = nc; self.es = es; self.E = {}
        for name, e in [("pe", nc.tensor), ("act", nc.scalar), ("dve", nc.vector), ("pool", nc.gpsimd), ("sp", nc.sync)]:
            self.E[name] = _Eng(name, e, es.enter_context(nc.semaphore("sem_" + name)))
        self.lastw = {}; self.reads = {}; self.streams = {}; self.nins = 0
    def sb(self, name, shape, dt, es=None):
        self.nins += 0; self._uid = getattr(self, "_uid", 0) + 1
        return (es or self.es).enter_context(self.nc.sbuf_tensor("s%d_%s" % (self._uid, name), list(shape), dt))
    def ps(self, name, shape, dt, es=None):
        self._uid = getattr(self, "_uid", 0) + 1
        return (es or self.es).enter_context(self.nc.psum_tensor("p%d_%s" % (self._uid, name), list(shape), dt))
    def _deps(self, r, w):
        ev = []
        for k in r:
            if k in self.lastw: ev.append(self.lastw[k])
        for k in w:
            if k in self.lastw: ev.append(self.lastw[k])
            ev.extend(self.reads.get(k, []))
        return ev
    def _needed(self, E, ev):
        need = {}
        for (sem, v, owner) in ev:
            if owner is E and (not SAME_ENGINE_SYNC or E.name == "pe"):
                continue
            key = id(sem)
            if E.seen.get(key, 0) < v and (key not in need or need[key][1] < v):
                need[key] = (sem, v)
        return list(need.values())
    def _wait(self, E, ev, keep_last=False):
        need = self._needed(E, ev)
        last = need.pop() if (keep_last and need) else None
        for (sem, v) in need:
            E.eng.wait_ge(sem, v); E.seen[id(sem)] = v
        return last
    def _record(self, r, w, event):
        for k in r:
            self.reads.setdefault(k, []).append(event)
        for k in w:
            self.lastw[k] = event; self.reads[k] = []
    def op(self, en, fn, r=(), w=()):
        E = self.E[en]
        last = self._wait(E, self._deps(r, w), keep_last=ATTACH_WAIT)
        ins = fn(E.eng)
        if last is not None:
            ins._wait_ge(last[0], last[1]); E.seen[id(last[0])] = last[1]
        E.count += 1
        ins.then_inc(E.sem, 1)
        self._record(r, w, (E.sem, E.count, E))
        self.nins += 1
        return ins
    def dma(self, en, out, in_, r=(), w=(), stream=None, **kw):
        E = self.E[en]
        if stream not in self.streams:
            self.streams[stream] = _Stream(self.es.enter_context(self.nc.semaphore("ds_" + str(stream))))
        S = self.streams[stream]
        self._wait(E, self._deps(r, w))
        ins = E.eng.dma_start(out=out, in_=in_, **kw)
        S.count += 1
        ins.then_inc(S.sem, 16)
        self._record(r, w, (S.sem, 16 * S.count, S))
        self.nins += 1
        return ins
    def all_events(self):
        ev = list(self.lastw.values())
        for l in self.reads.values(): ev.extend(l)
        return ev
    def barrier(self):
        ev = self.all_events()
        best = {}
        for (sem, v, o) in ev:
            if id(sem) not in best or best[id(sem)][1] < v: best[id(sem)] = (sem, v, None)
        for E in self.E.values():
            self._wait(E, list(best.values()))
        self.lastw = {}; self.reads = {}

class Cfg:
    def __init__(self, seq=16384):
        self.SEQ = seq; self.QTR = seq // 4; self.SLOC = seq
        self.TB = 512; self.NBLK = self.SLOC // 512; self.OWNBLK = self.QTR // 512
        self.NT = self.SLOC // 128; self.T0 = 3 * self.QTR // 128

def _t5_bucket(rel):
    n = np.maximum(rel, 0)
    nf = np.maximum(n, 1).astype(np.float32)
    large = 16 + (np.log(nf / np.float32(16)) / np.float32(math.log(128 / 16)) * np.float32(16)).astype(np.int32)
    large = np.minimum(large, 31)
    return np.where(n < 16, n, large)

def _col16(v):
    return np.ascontiguousarray(np.asarray(v, np.float32).reshape(16, 128).T)

def prep_inputs(inp, cfg):
    f = lambda a: np.ascontiguousarray(np.asarray(a, np.float32))
    x = f(inp["x"]); B = x.shape[0]
    QTR, SLOC = cfg.QTR, cfg.SLOC
    shared = {}
    shared["ada_w"] = f(inp["ada_w"][0])
    shared["ada_bT"] = np.ascontiguousarray(f(inp["ada_b"][0]).reshape(96, 128).T)
    shared["ada_brow"] = f(inp["ada_b"][0]).reshape(1, 6 * D)
    shared["n1T"] = _col16(inp["norm1_w"][0]); shared["n2T"] = _col16(inp["norm2_w"][0])
    shared["w_in"] = f(inp["w_in"][0]); shared["w_out"] = f(inp["w_out"][0]); shared["peer_wq"] = f(inp["peer_wq"][0])
    shared["qnw"] = np.tile(f(inp["q_norm_w"][0]), 2).reshape(128, 1)
    shared["knw"] = np.tile(f(inp["k_norm_w"][0]), 2).reshape(128, 1)
    rb = f(inp["rel_bias"])
    shared["cb31"] = np.ascontiguousarray(np.broadcast_to(rb[31].reshape(1, 16), (128, 16)))
    kk = np.arange(128)[:, None]; qq = np.arange(128)[None, :]
    bd = rb[_t5_bucket(qq - kk)]
    bp = rb[_t5_bucket(qq - kk + 128)]
    shared["bdiag"] = np.ascontiguousarray(bd.reshape(128, 128, 16).transpose(0, 2, 1))
    shared["bprev"] = np.ascontiguousarray(bp.reshape(128, 128, 16).transpose(0, 2, 1))
    shared["cmask"] = np.where(qq >= kk, 0.0, -30000.0).astype(np.float32)
    shared["lam4"] = np.concatenate([f(inp["lambda_q1"][0]), f(inp["lambda_k1"][0]), f(inp["lambda_q2"][0]), f(inp["lambda_k2"][0])]).reshape(1, 256)
    shared["subln"] = f(inp["subln_w"][0]).reshape(1, 128)
    cw = f(inp["conv_w"][0])
    shared["convwT"] = np.ascontiguousarray(cw.T.reshape(12, 128, 4).transpose(1, 0, 2))
    shared["convb"] = np.ascontiguousarray(f(inp["conv_b"][0]).reshape(12, 128).T)
    shared["dtb"] = f(inp["dt_bias"][0]).reshape(1, 16); shared["alog"] = f(inp["a_log"][0]).reshape(1, 16)
    shared["dsk"] = f(inp["d_skip"][0]).reshape(1, 16)
    shared["ssmnw"] = f(inp["ssm_norm_w"][0]).reshape(1, 1024)
    pk = f(inp["peer_keys"][0]).reshape(16, 128, 128)
    shared["keysT"] = np.ascontiguousarray(pk.transpose(2, 0, 1))
    shared["downT"] = np.ascontiguousarray(f(inp["expert_down"][0]).T)
    shared["up"] = f(inp["expert_up"][0])
    shared["ident"] = np.eye(128, dtype=np.float32)
    shared["tri"] = (kk <= qq).astype(np.float32)
    bo = np.zeros((128, 128), np.float32); bo[:64, :64] = 1; bo[64:, 64:] = 1
    shared["blockones"] = bo
    maps = []
    for core in range(8):
        b, j = core // 4, core % 4
        m = dict(shared)
        xl = np.zeros((SLOC, D), np.float32)
        n = (j + 1) * QTR
        xl[SLOC - n:] = x[b, :n]
        m["xloc"] = xl
        vc = np.zeros((128, cfg.NBLK), np.float32); vc[:, cfg.NBLK - (j + 1) * cfg.OWNBLK:] = 1.0
        m["validcol"] = vc
        m["ccol"] = _col16(inp["c"][b])
        maps.append(m)
    return maps

IN_SHAPES = lambda cfg: {
    "xloc": [cfg.SLOC, D], "validcol": [128, cfg.NBLK], "ccol": [128, 16],
    "ada_w": [D, 6 * D], "ada_bT": [128, 96], "ada_brow": [1, 6 * D], "n1T": [128, 16], "n2T": [128, 16],
    "w_in": [D, 5648], "w_out": [D, D], "peer_wq": [D, D], "qnw": [128, 1], "knw": [128, 1],
    "cb31": [128, 16], "bdiag": [128, 16, 128], "bprev": [128, 16, 128], "cmask": [128, 128],
    "lam4": [1, 256], "subln": [1, 128], "convwT": [128, 12, 4], "convb": [128, 12],
    "dtb": [1, 16], "alog": [1, 16], "dsk": [1, 16], "ssmnw": [1, 1024], "keysT": [128, 16, 128],
    "downT": [D, NEXP], "up": [NEXP, D], "ident": [128, 128], "tri": [128, 128], "blockones": [128, 128],
}

def build(cfg, debug=False, stop_after=99):
    nc = bass.Bass("TRN2", target_bir_lowering=False)
    I = {k: nc.dram_tensor(k, s, F32, kind="ExternalInput").ap() for k, s in IN_SHAPES(cfg).items()}
    y_out = nc.dram_tensor("y", [cfg.QTR, D], F32, kind="ExternalOutput").ap()
    skind = "ExternalOutput" if debug else "Internal"
    def scratch(name, shape, dt):
        return nc.dram_tensor(name, list(shape), dt, kind=skind).ap()
    S = {}
    S["w_in_bf"] = scratch("w_in_bf", [D, 5648], BF16)
    S["w_out_bf"] = scratch("w_out_bf", [D, D], BF16)
    S["wq_bf"] = scratch("wq_bf", [D, D], BF16)
    S["downT_bf"] = scratch("downT_bf", [D, NEXP], BF16)
    S["up_bf"] = scratch("up_bf", [NEXP, D], BF16)
    S["kT"] = scratch("kT_d", [H, 128, cfg.SLOC], BF16)
    S["qT"] = scratch("qT_d", [H, 128, cfg.QTR], BF16)
    S["v"] = scratch("v_d", [H, 128, cfg.NT, 129], BF16)
    S["z"] = scratch("z_d", [cfg.QTR, 1024], BF16)
    S["uT"] = scratch("uT_d", [12, 128, cfg.SLOC], F32)
    S["dt"] = scratch("dt_d", [cfg.SLOC, 16], F32)
    S["mix"] = scratch("mix_d", [cfg.QTR, D], BF16)
    S["h"] = scratch("h_d", [cfg.QTR, D], F32)
    S["G"] = scratch("G_d", [cfg.QTR, NEXP], BF16)
    S["mod"] = scratch("mod_d", [128, 96], F32)
    es = contextlib.ExitStack()
    with es:
        c = Ctx(nc, es)
        P = {}
        phase0(c, cfg, I, S, P)
        if stop_after >= 1: phase_cast(c, cfg, I, S, P, stop_after)
        if stop_after >= 2: phase1a(c, cfg, I, S, P)
        if stop_after >= 3: phase1b(c, cfg, I, S, P)
        if stop_after >= 4: phase_attn(c, cfg, I, S, P)
        if stop_after >= 5: phase_out(c, cfg, I, S, P, y_out)
        if stop_after < 5:
            with contextlib.ExitStack() as pes:
                zt = c.sb("zt_dbg", [128, D], F32, pes)
                c.op("dve", lambda e: e.memset(zt[:], 0.0), w=["zt"])
                for i in range(cfg.QTR // 128):
                    c.dma("sp", y_out[i * 128:(i + 1) * 128, :], zt[:], r=["zt"], stream="yo")
                c.barrier()
        c.barrier()
    return nc


def phase0(c, cfg, I, S, P):
    es = c.es
    PERSIST = {"identf": [128, 128], "tri": [128, 128], "validcol": [128, cfg.NBLK], "qnw": [128, 1], "cb31": [128, 16],
               "bdiag": [128, 16, 128], "bprev": [128, 16, 128], "subln": [128, 128], "convwT": [128, 12, 4], "convb": [128, 12],
               "dtb": [128, 16], "dsk": [128, 16], "ssmnw": [128, 1024]}
    pre = {k: c.sb(k, sh, F32) for k, sh in PERSIST.items()}
    for k, sh, dt in (("ident", [128, 128], BF16), ("blockones", [128, 128], BF16), ("ones", [128, 128], F32), ("knw8", [128, 1], F32),
                      ("keysT", [128, 16, 128], BF16), ("aneg", [128, 16], F32), ("neglam", [128, 1], F32), ("g1b", [128, D], F32), ("g2b", [128, D], F32),
                      ("w1p", [128, 16], F32), ("sh1T", [128, 16], F32), ("w2p", [128, 16], F32), ("sh2T", [128, 16], F32)):
        pre[k] = c.sb(k, sh, dt)
    tes = contextlib.ExitStack()
    def load(name, shape, src, eng="sp", dt=F32):
        t = pre[name] if name in pre else c.sb(name, shape, dt, tes)
        c.dma(eng, t[:], src, w=[name], stream="ld_" + name)
        return t
    _sb0 = c.sb
    def _sb(name, shape, dt, es_=None):
        if name in pre: return pre[name]
        return _sb0(name, shape, dt, es_ or tes)
    c.sb = _sb
    P["identf"] = load("identf", [128, 128], I["ident"])
    P["tri"] = load("tri", [128, 128], I["tri"])
    bof = load("bof", [128, 128], I["blockones"])
    P["ident"] = c.sb("ident", [128, 128], BF16); P["blockones"] = c.sb("blockones", [128, 128], BF16)
    c.op("dve", lambda e: e.tensor_copy(P["ident"][:], P["identf"][:]), r=["identf"], w=["ident"])
    c.op("dve", lambda e: e.tensor_copy(P["blockones"][:], bof[:]), r=["bof"], w=["blockones"])
    P["ones"] = c.sb("ones", [128, 128], F32)
    c.op("dve", lambda e: e.memset(P["ones"][:], 1.0), w=["ones"])
    P["validcol"] = load("validcol", [128, cfg.NBLK], I["validcol"])
    ccol = load("ccol", [128, 16], I["ccol"])
    adabT = load("adabT", [128, 96], I["ada_bT"])
    n1T = load("n1T", [128, 16], I["n1T"]); n2T = load("n2T", [128, 16], I["n2T"])
    P["qnw"] = load("qnw", [128, 1], I["qnw"]); knw = load("knw", [128, 1], I["knw"])
    P["knw8"] = c.sb("knw8", [128, 1], F32)
    c.op("dve", lambda e: e.tensor_scalar(out=P["knw8"][:], in0=knw[:], scalar1=8.0, scalar2=None, op0=ALU.mult), r=["knw"], w=["knw8"])
    P["cb31"] = load("cb31", [128, 16], I["cb31"])
    P["bdiag"] = load("bdiag", [128, 16, 128], I["bdiag"]); P["bprev"] = load("bprev", [128, 16, 128], I["bprev"])
    cmask = load("cmask", [128, 128], I["cmask"])
    c.op("dve", lambda e: e.tensor_tensor(out=P["bdiag"][:], in0=P["bdiag"][:], in1=cmask[:].unsqueeze(1).to_broadcast([128, 16, 128]), op=ALU.add), r=["bdiag", "cmask"], w=["bdiag"])
    for bk in ("bdiag", "bprev"):
        c.op("dve", lambda e: e.tensor_tensor(out=P[bk][:], in0=P[bk][:], in1=P["cb31"][:].unsqueeze(2).to_broadcast([128, 16, 128]), op=ALU.subtract), r=[bk, "cb31"], w=[bk])
    lam4 = load("lam4", [128, 256], I["lam4"].partition_broadcast(128))
    P["subln"] = load("subln", [128, 128], I["subln"].partition_broadcast(128))
    c.op("dve", lambda e: e.tensor_scalar(out=P["subln"][:], in0=P["subln"][:], scalar1=1.0 - LAM_INIT, scalar2=None, op0=ALU.mult), r=["subln"], w=["subln"])
    P["convwT"] = load("convwT", [128, 12, 4], I["convwT"]); P["convb"] = load("convb", [128, 12], I["convb"])
    P["dtb"] = load("dtb", [128, 16], I["dtb"].partition_broadcast(128))
    alog = load("alog", [128, 16], I["alog"].partition_broadcast(128))
    P["dsk"] = load("dsk", [128, 16], I["dsk"].partition_broadcast(128))
    P["ssmnw"] = load("ssmnw", [128, 1024], I["ssmnw"].partition_broadcast(128))
    keysf = load("keysf", [128, 16, 128], I["keysT"])
    P["keysT"] = c.sb("keysT", [128, 16, 128], BF16)
    c.op("dve", lambda e: e.tensor_copy(P["keysT"][:], keysf[:]), r=["keysf"], w=["keysT"])
    P["aneg"] = c.sb("aneg", [128, 16], F32)
    c.op("act", lambda e: e.activation(out=P["aneg"][:], in_=alog[:], func=AF.Exp), r=["alog"], w=["aneg"])
    c.op("dve", lambda e: e.tensor_scalar(out=P["aneg"][:], in0=P["aneg"][:], scalar1=-1.0, scalar2=None, op0=ALU.mult), r=["aneg"], w=["aneg"])
    lp = c.sb("lp", [128, 128], F32); ls = c.sb("ls", [128, 2], F32)
    c.op("dve", lambda e: e.tensor_tensor(out=lp[:, 0:64], in0=lam4[:, 0:64], in1=lam4[:, 64:128], op=ALU.mult), r=["lam4"], w=["lp"])
    c.op("dve", lambda e: e.tensor_tensor(out=lp[:, 64:128], in0=lam4[:, 128:192], in1=lam4[:, 192:256], op=ALU.mult), r=["lam4", "lp"], w=["lp"])
    c.op("dve", lambda e: e.tensor_reduce(out=ls[:], in_=lp[:].rearrange("p (a b) -> p a b", a=2), axis=AX.X, op=ALU.add), r=["lp"], w=["ls"])
    c.op("act", lambda e: e.activation(out=ls[:], in_=ls[:], func=AF.Exp), r=["ls"], w=["ls"])
    P["neglam"] = c.sb("neglam", [128, 1], F32)
    c.op("dve", lambda e: e.tensor_tensor(out=P["neglam"][:], in0=ls[:, 1:2], in1=ls[:, 0:1], op=ALU.subtract), r=["ls"], w=["neglam"])
    c.op("dve", lambda e: e.tensor_scalar(out=P["neglam"][:], in0=P["neglam"][:], scalar1=-LAM_INIT, scalar2=None, op0=ALU.add), r=["neglam"], w=["neglam"])
    scf = c.sb("scf", [128, 16], F32)
    c.op("act", lambda e: e.activation(out=scf[:], in_=ccol[:], func=AF.Silu), r=["ccol"], w=["scf"])
    modT = c.sb("modT", [128, 96], F32)
    P["g1b"] = c.sb("g1b", [128, D], F32); P["g2b"] = c.sb("g2b", [128, D], F32)
    with contextlib.ExitStack() as pes:
        aw = [c.sb("aw%d" % i, [128, 16, 512], F32, pes) for i in range(2)]
        abr = [c.sb("abr%d" % i, [128, 512], F32, pes) for i in range(2)]
        psm = [c.ps("psm%d" % i, [128, 512], F32, pes) for i in range(2)]
        awv = I["ada_w"].rearrange("(c p) n -> p c n", p=128)
        for cb in range(24):
            bi = cb % 2
            c.dma("sp", aw[bi][:], awv[:, :, cb * 512:(cb + 1) * 512], w=["aw%d" % bi], stream="aw%d" % bi)
            grp = cb // 4
            if grp in (2, 5):
                c.dma("pool", abr[bi][:], I["ada_brow"][:, cb * 512:(cb + 1) * 512].partition_broadcast(128), w=["abr%d" % bi], stream="abr%d" % bi)
                for kc in range(16):
                    c.op("pe", lambda e: e.matmul(psm[bi][:], scf[:, kc:kc + 1].to_broadcast([128, 128]), aw[bi][:, kc, :], start=(kc == 0), stop=(kc == 15)),
                         r=["scf", "aw%d" % bi], w=["psm%d" % bi])
                dst = P["g1b"] if grp == 2 else P["g2b"]; dk = "g1b" if grp == 2 else "g2b"
                off = (cb % 4) * 512
                c.op("dve", lambda e: e.tensor_tensor(out=dst[:, off:off + 512], in0=psm[bi][:], in1=abr[bi][:], op=ALU.add), r=["psm%d" % bi, "abr%d" % bi, dk], w=[dk])
            else:
                for cc in range(4):
                    for kc in range(16):
                        c.op("pe", lambda e: e.matmul(psm[bi][:, cc:cc + 1], aw[bi][:, kc, cc * 128:(cc + 1) * 128], scf[:, kc:kc + 1], start=(kc == 0), stop=(kc == 15)),
                             r=["scf", "aw%d" % bi], w=["psm%d" % bi])
                c.op("dve", lambda e: e.tensor_tensor(out=modT[:, cb * 4:cb * 4 + 4], in0=psm[bi][:, 0:4], in1=adabT[:, cb * 4:cb * 4 + 4], op=ALU.add), r=["psm%d" % bi, "adabT", "modT"], w=["modT"])
        c.dma("sp", S["mod"], modT[:], r=["modT"], stream="modo")
        c.barrier()
    P["w1p"] = c.sb("w1p", [128, 16], F32); P["sh1T"] = c.sb("sh1T", [128, 16], F32)
    P["w2p"] = c.sb("w2p", [128, 16], F32); P["sh2T"] = c.sb("sh2T", [128, 16], F32)
    for (wp, sh, nT, nk, o) in ((P["w1p"], P["sh1T"], n1T, "n1T", 0), (P["w2p"], P["sh2T"], n2T, "n2T", 48)):
        c.op("dve", lambda e: e.tensor_scalar(out=wp[:], in0=modT[:, o + 16:o + 32], scalar1=1.0, scalar2=None, op0=ALU.add), r=["modT"], w=["wp%d" % o])
        c.op("dve", lambda e: e.tensor_tensor(out=wp[:], in0=wp[:], in1=nT[:], op=ALU.mult), r=["wp%d" % o, nk], w=["wp%d" % o])
        c.op("dve", lambda e: e.tensor_copy(sh[:], modT[:, o:o + 16]), r=["modT"], w=["sh%d" % o])
    c.barrier()
    c.sb = _sb0
    tes.close()


def cast_dram(c, src, dst, R, C, tag):
    with contextlib.ExitStack() as pes:
        CW = min(C, 2048)
        fin = [c.sb("cf%d" % i, [128, CW], F32, pes) for i in range(3)]
        fo = [c.sb("co%d" % i, [128, CW], BF16, pes) for i in range(3)]
        n = 0
        for r0 in range(0, R, 128):
            for c0 in range(0, C, CW):
                cw = min(CW, C - c0); bi = n % 3
                c.dma("sp", fin[bi][:, :cw], src[r0:r0 + 128, c0:c0 + cw], w=["cf%d" % bi], stream="cf%d" % bi)
                if n % 2 == 0:
                    c.op("dve", lambda e: e.tensor_copy(fo[bi][:, :cw], fin[bi][:, :cw]), r=["cf%d" % bi], w=["co%d" % bi])
                else:
                    c.op("act", lambda e: e.activation(out=fo[bi][:, :cw], in_=fin[bi][:, :cw], func=AF.Copy), r=["cf%d" % bi], w=["co%d" % bi])
                c.dma("pool", dst[r0:r0 + 128, c0:c0 + cw], fo[bi][:, :cw], r=["co%d" % bi], w=[tag], stream="co%d" % bi)
                n += 1
        c.barrier()


def phase_cast(c, cfg, I, S, P, stop_after):
    cast_dram(c, I["w_in"], S["w_in_bf"], D, 5648, "w_in_bf")
    if stop_after >= 5:
        cast_dram(c, I["w_out"], S["w_out_bf"], D, D, "w_out_bf")
        cast_dram(c, I["peer_wq"], S["wq_bf"], D, D, "wq_bf")
        cast_dram(c, I["downT"], S["downT_bf"], D, NEXP, "downT_bf")
        cast_dram(c, I["up"], S["up_bf"], NEXP, D, "up_bf")


def norm_transpose(c, pes_bufs, x_src_rows, wp, sh, wpk, shk, hnT, hnTk, tag):
    xt, junk, xs, ssq, pst, P = pes_bufs
    for i in range(4):
        bi = i % 2
        c.dma("sp", xt[bi][:], x_src_rows[i * 128:(i + 1) * 128, :], w=["xt%d" % bi], stream="xt%d" % bi)
        c.op("act", lambda e: e.activation(out=junk[:], in_=xt[bi][:], func=AF.Square, accum_out=ssq[:, i:i + 1]), r=["xt%d" % bi], w=["junk", "ssq"])
        c.op("dve", lambda e: e.tensor_scalar(out=ssq[:, i:i + 1], in0=ssq[:, i:i + 1], scalar1=1.0 / D, scalar2=EPS, op0=ALU.mult, op1=ALU.add), r=["ssq"], w=["ssq"])
        c.op("act", lambda e: e.activation(out=ssq[:, i:i + 1], in_=ssq[:, i:i + 1], func=AF.Sqrt), r=["ssq"], w=["ssq"])
        c.op("dve", lambda e: e.reciprocal(out=ssq[:, i:i + 1], in_=ssq[:, i:i + 1]), r=["ssq"], w=["ssq"])
        c.op("act", lambda e: e.activation(out=xs[:, i, :], in_=xt[bi][:], func=AF.Copy, scale=ssq[:, i:i + 1]), r=["xt%d" % bi, "ssq"], w=["xs"])
    for cp in range(8):
        pb = cp % 2
        for j in range(2):
            ch = cp * 2 + j
            for i in range(4):
                c.op("pe", lambda e: e.transpose(pst[pb][:, j * 512 + i * 128: j * 512 + (i + 1) * 128], xs[:, i, ch * 128:(ch + 1) * 128], P["ident"][:]),
                     r=["xs", "ident"], w=["pst%d" % pb])
        for j in range(2):
            ch = cp * 2 + j
            c.op("act", lambda e: e.activation(out=hnT[:, ch, :], in_=pst[pb][:, j * 512:(j + 1) * 512], func=AF.Identity, scale=wp[:, ch:ch + 1], bias=sh[:, ch:ch + 1]),
                 r=["pst%d" % pb, wpk, shk], w=[hnTk])


def phase1a(c, cfg, I, S, P):
    with contextlib.ExitStack() as pes:
        xt = [c.sb("xt%d" % i, [128, D], F32, pes) for i in range(2)]
        junk = c.sb("junk", [128, D], BF16, pes)
        xs = c.sb("xs", [128, 4, D], BF16, pes)
        ssq = c.sb("ssq", [128, 4], F32, pes)
        pst = [c.ps("pst%d" % i, [128, 1024], BF16, pes) for i in range(2)]
        hnTs = [c.sb("hnT%d" % i, [128, 16, 512], BF16, pes) for i in range(2)]
        wg = [c.sb("wg%d" % i, [128, 16, 512], BF16, pes) for i in range(2)]
        wdt = c.sb("wdt", [128, 16, 16], BF16, pes)
        psa = [c.ps("psa%d" % i, [128, 512], F32, pes) for i in range(3)]
        ps2 = c.ps("ps2", [128, 512], F32, pes)
        sq = c.sb("sq", [128, 512], BF16, pes); rs = c.sb("rs", [128, 512], F32, pes)
        ko = [c.sb("ko%d" % i, [128, 512], BF16, pes) for i in range(2)]
        vo = c.sb("vo", [128, 8, 4, 129], BF16, pes)
        zo = c.sb("zo", [128, 4, 1024], BF16, pes)
        uo = [c.sb("uo%d" % i, [128, 512], F32, pes) for i in range(2)]
        dto = c.sb("dto", [128, 4, 16], F32, pes)
        wv = S["w_in_bf"].rearrange("(c p) n -> p c n", p=128)
        c.dma("sp", wdt[:], wv[:, :, 5632:5648], r=["w_in_bf"], w=["wdt"], stream="wdt")
        bufs = (xt, junk, xs, ssq, pst, P)
        state = {"wn": 0, "pn": 0, "kn": 0, "un": 0}
        def load_w(col0):
            bi = state["wn"] % 2; state["wn"] += 1
            c.dma("sp", wg[bi][:], wv[:, :, col0:col0 + 512], r=["w_in_bf"], w=["wg%d" % bi], stream="wg%d" % bi)
            return wg[bi], "wg%d" % bi
        def next_ps():
            bi = state["pn"] % 3; state["pn"] += 1
            return psa[bi], "psa%d" % bi
        def NT(b_):
            norm_transpose(c, bufs, I["xloc"][b_ * 512:(b_ + 1) * 512, :], P["w1p"], P["sh1T"], "wp0", "sh0", hnTs[b_ % 2], "hnT%d" % (b_ % 2), "a")
        NT(0)
        for blk in range(cfg.NBLK):
            own = blk >= cfg.NBLK - cfg.OWNBLK
            oblk = blk - (cfg.NBLK - cfg.OWNBLK)
            vcol = P["validcol"][:, blk:blk + 1]
            if blk + 1 < cfg.NBLK: NT(blk + 1)
            hnT = hnTs[blk % 2]; HK = "hnT%d" % (blk % 2)
            fm = [("k", 1024), ("k", 1536)] + ([("q", 0), ("q", 512)] if own else [])
            for (kind, col0) in fm:
                w, wk = load_w(col0)
                for cc in range(4):
                    hh = (col0 % 1024) // 128 + cc
                    ps, pk = next_ps()
                    for kc in range(16):
                        c.op("pe", lambda e: e.matmul(ps[:], w[:, kc, cc * 128:(cc + 1) * 128], hnT[:, kc, :], start=(kc == 0), stop=(kc == 15)), r=[wk, HK], w=[pk])
                    c.op("act", lambda e: e.activation(out=sq[:], in_=ps[:], func=AF.Square), r=[pk], w=["sq"])
                    c.op("pe", lambda e: e.matmul(ps2[:], P["blockones"][:], sq[:], start=True, stop=True), r=["sq", "blockones"], w=["ps2"])
                    c.op("dve", lambda e: e.tensor_scalar(out=rs[:], in0=ps2[:], scalar1=64.0 * EPS, scalar2=None, op0=ALU.add), r=["ps2"], w=["rs"])
                    c.op("act", lambda e: e.activation(out=rs[:], in_=rs[:], func=AF.Sqrt), r=["rs"], w=["rs"])
                    c.op("dve", lambda e: e.reciprocal(out=rs[:], in_=rs[:]), r=["rs"], w=["rs"])
                    ki = state["kn"] % 2; state["kn"] += 1
                    wn = P["knw8"] if kind == "k" else P["qnw"]
                    c.op("dve", lambda e: e.scalar_tensor_tensor(out=ko[ki][:], in0=ps[:], scalar=wn[:, 0:1], in1=rs[:], op0=ALU.mult, op1=ALU.mult),
                         r=[pk, "rs", "knw8", "qnw"], w=["ko%d" % ki])
                    if kind == "k":
                        c.dma("pool", S["kT"][hh, :, blk * 512:(blk + 1) * 512], ko[ki][:], r=["ko%d" % ki], w=["kT_d"], stream="ko%d" % ki)
                    else:
                        c.dma("pool", S["qT"][hh, :, oblk * 512:(oblk + 1) * 512], ko[ki][:], r=["ko%d" % ki], w=["qT_d"], stream="ko%d" % ki)
            for g in range(3):
                w, wk = load_w(4096 + g * 512)
                for cc in range(4):
                    ch = g * 4 + cc
                    ps, pk = next_ps()
                    for kc in range(16):
                        c.op("pe", lambda e: e.matmul(ps[:], w[:, kc, cc * 128:(cc + 1) * 128], hnT[:, kc, :], start=(kc == 0), stop=(kc == 15)), r=[wk, HK], w=[pk])
                    ui = state["un"] % 2; state["un"] += 1
                    c.op("dve", lambda e: e.tensor_scalar(out=uo[ui][:], in0=ps[:], scalar1=vcol, scalar2=None, op0=ALU.mult), r=[pk, "validcol"], w=["uo%d" % ui])
                    c.dma("pool", S["uT"][ch, :, blk * 512:(blk + 1) * 512], uo[ui][:], r=["uo%d" % ui], w=["uT_d"], stream="uo%d" % ui)
            c.op("dve", lambda e: e.tensor_copy(vo[:, :, :, 128:129].rearrange("p a b c -> p (a b c)"), vcol.to_broadcast([128, 32])), r=["validcol"], w=["vo"])
            for g in range(2):
                w, wk = load_w(2048 + g * 512)
                for i in range(4):
                    ps, pk = next_ps()
                    for kc in range(16):
                        c.op("pe", lambda e: e.matmul(ps[:], hnT[:, kc, i * 128:(i + 1) * 128], w[:, kc, :], start=(kc == 0), stop=(kc == 15)), r=[wk, HK], w=[pk])
                    c.op("dve", lambda e: e.tensor_scalar(out=vo[:, g * 4:(g + 1) * 4, i, 0:128], in0=ps[:].rearrange("p (a b) -> p a b", a=4), scalar1=vcol, scalar2=None, op0=ALU.mult),
                         r=[pk, "validcol", "vo"], w=["vo"])
            for hh in range(8):
                c.dma("pool", S["v"][hh, :, blk * 4:(blk + 1) * 4, :], vo[:, hh, :, :], r=["vo"], w=["v_d"], stream="vo")
            ps, pk = next_ps()
            for i in range(4):
                for kc in range(16):
                    c.op("pe", lambda e: e.matmul(ps[:, i * 16:(i + 1) * 16], hnT[:, kc, i * 128:(i + 1) * 128], wdt[:, kc, :], start=(kc == 0), stop=(kc == 15)), r=["wdt", HK], w=[pk])
            c.op("dve", lambda e: e.tensor_copy(dto[:].rearrange("p a b -> p (a b)"), ps[:, 0:64]), r=[pk], w=["dto"])
            c.dma("pool", S["dt"][blk * 512:(blk + 1) * 512, :].rearrange("(n p) c -> p n c", p=128), dto[:], r=["dto"], w=["dt_d"], stream="dto")
            if own:
                for g in range(2):
                    w, wk = load_w(3072 + g * 512)
                    for i in range(4):
                        ps, pk = next_ps()
                        for kc in range(16):
                            c.op("pe", lambda e: e.matmul(ps[:], hnT[:, kc, i * 128:(i + 1) * 128], w[:, kc, :], start=(kc == 0), stop=(kc == 15)), r=[wk, HK], w=[pk])
                        c.op("act", lambda e: e.activation(out=zo[:, i, g * 512:(g + 1) * 512], in_=ps[:], func=AF.Silu), r=[pk, "zo"], w=["zo"])
                c.dma("pool", S["z"][oblk * 512:(oblk + 1) * 512, :].rearrange("(n p) c -> p n c", p=128), zo[:], r=["zo"], w=["z_d"], stream="zo")
        c.barrier()


def phase1b(c, cfg, I, S, P):
    with contextlib.ExitStack() as pes:
        U = c.sb("U", [128, 12, 515], F32, pes)
        acc = [c.sb("acc%d" % i, [128, 512], F32, pes) for i in range(2)]
        xbcT = c.sb("xbcT", [128, 12, 512], BF16, pes)
        state_f = c.sb("state_f", [128, 2, 512], F32, pes); state_b = c.sb("state_b", [128, 2, 512], BF16, pes)
        dtr = c.sb("dtr", [128, 4, 16], F32, pes); dtv = c.sb("dtv", [128, 4, 16], F32, pes); dta = c.sb("dta", [128, 4, 16], F32, pes)
        acs = c.sb("acs", [128, 32], F32, pes); ein = c.sb("ein", [128, 48], F32, pes); dec = c.sb("dec", [128, 48], F32, pes)
        xtok = c.sb("xtok", [128, 1024], BF16, pes); xc = c.sb("xc", [128, 1024], BF16, pes); xcd = c.sb("xcd", [128, 1024], BF16, pes)
        Btok = c.sb("Btok", [128, 2, 128], BF16, pes)
        cbm = c.sb("cbm", [128, 2, 128], F32, pes)
        diag = c.sb("diag", [128, 16, 128], F32, pes); seg = c.sb("seg", [128, 16, 128], F32, pes)
        Mt = c.sb("Mt", [128, 16, 128], BF16, pes)
        t1 = c.sb("t1", [128, 1024], F32, pes); t2 = c.sb("t2", [128, 1024], F32, pes)
        zs = c.sb("zs", [128, 1024], BF16, pes); yo = c.sb("yo", [128, 1024], BF16, pes)
        junk2 = c.sb("junk2", [128, 512], BF16, pes); ssg = c.sb("ssg", [128, 2], F32, pes)
        b0 = c.ps("b0", [128, 512], F32, pes)
        b1 = c.ps("b1", [128, 1024], BF16, pes); b2 = c.ps("b2", [128, 1024], BF16, pes)
        b34 = [c.ps("b3", [128, 512], F32, pes), c.ps("b4", [128, 512], F32, pes)]
        b56 = [c.ps("b5", [128, 512], F32, pes), c.ps("b6", [128, 512], F32, pes)]
        b7 = c.ps("b7", [128, 512], F32, pes)
        c.op("dve", lambda e: e.memset(U[:, :, 0:3], 0.0), w=["U"])
        c.op("dve", lambda e: e.memset(state_f[:], 0.0), w=["state_f"])
        c.op("dve", lambda e: e.memset(state_b[:], 0.0), w=["state_b"])
        for blk in range(cfg.NBLK):
            own = blk >= cfg.NBLK - cfg.OWNBLK
            oblk = blk - (cfg.NBLK - cfg.OWNBLK)
            vcol = P["validcol"][:, blk:blk + 1]
            c.dma("sp", U[:, :, 3:515], S["uT"][:, :, blk * 512:(blk + 1) * 512].rearrange("c p t -> p c t"), r=["uT_d"], w=["U"], stream="U")
            c.dma("sp", dtr[:], S["dt"][blk * 512:(blk + 1) * 512, :].rearrange("(n p) c -> p n c", p=128), r=["dt_d"], w=["dtr"], stream="dtr")
            for ch in range(12):
                a = acc[ch % 2]; ak = "acc%d" % (ch % 2)
                c.op("act", lambda e: e.activation(out=a[:], in_=U[:, ch, 0:512], func=AF.Identity, scale=P["convwT"][:, ch, 0:1], bias=P["convb"][:, ch:ch + 1]),
                     r=["U", "convwT", "convb"], w=[ak])
                for k in range(1, 4):
                    c.op("dve", lambda e: e.scalar_tensor_tensor(out=a[:], in0=U[:, ch, k:k + 512], scalar=P["convwT"][:, ch, k:k + 1], in1=a[:], op0=ALU.mult, op1=ALU.add),
                         r=["U", "convwT", ak], w=[ak])
                c.op("act", lambda e: e.activation(out=xbcT[:, ch, :], in_=a[:], func=AF.Silu), r=[ak], w=["xbcT"])
            c.op("pool", lambda e: e.tensor_copy(U[:, :, 0:3], U[:, :, 512:515]), r=["U"], w=["U"])
            c.op("dve", lambda e: e.tensor_tensor(out=dtv[:], in0=dtr[:], in1=P["dtb"][:].unsqueeze(1).to_broadcast([128, 4, 16]), op=ALU.add), r=["dtr", "dtb"], w=["dtv"])
            c.op("act", lambda e: e.activation(out=dtv[:], in_=dtv[:], func=AF.Exp), r=["dtv"], w=["dtv"])
            c.op("act", lambda e: e.activation(out=dtv[:], in_=dtv[:], func=AF.Ln, bias=1.0), r=["dtv"], w=["dtv"])
            c.op("dve", lambda e: e.tensor_scalar(out=dtv[:], in0=dtv[:], scalar1=vcol, scalar2=None, op0=ALU.mult), r=["dtv", "validcol"], w=["dtv"])
            c.op("dve", lambda e: e.tensor_tensor(out=dta[:], in0=dtv[:], in1=P["aneg"][:].unsqueeze(1).to_broadcast([128, 4, 16]), op=ALU.mult), r=["dtv", "aneg"], w=["dta"])
            for i in range(4):
                ts = slice(i * 128, (i + 1) * 128)
                c.op("pe", lambda e: e.matmul(b0[:, 0:16], P["tri"][:], dta[:, i, :], start=True, stop=True), r=["tri", "dta"], w=["b0"])
                c.op("pe", lambda e: e.matmul(b0[:, 16:32], P["ones"][:], dta[:, i, :], start=True, stop=True), r=["ones", "dta", "b0"], w=["b0"])
                c.op("dve", lambda e: e.tensor_copy(acs[:], b0[:, 0:32]), r=["b0"], w=["acs"])
                c.op("dve", lambda e: e.tensor_tensor(out=ein[:, 0:16], in0=acs[:, 16:32], in1=acs[:, 0:16], op=ALU.subtract), r=["acs"], w=["ein"])
                c.op("dve", lambda e: e.tensor_copy(ein[:, 16:32], acs[:, 16:32]), r=["acs", "ein"], w=["ein"])
                c.op("dve", lambda e: e.tensor_copy(ein[:, 32:48], acs[:, 0:16]), r=["acs", "ein"], w=["ein"])
                c.op("act", lambda e: e.activation(out=dec[:], in_=ein[:], func=AF.Exp), r=["ein"], w=["dec"])
                for ch in range(8):
                    c.op("pe", lambda e: e.transpose(b1[:, ch * 128:(ch + 1) * 128], xbcT[:, ch, ts], P["ident"][:]), r=["xbcT", "ident"], w=["b1"])
                for g in range(2):
                    c.op("pe", lambda e: e.transpose(b2[:, g * 128:(g + 1) * 128], xbcT[:, 8 + g, ts], P["ident"][:]), r=["xbcT", "ident"], w=["b2"])
                c.op("act", lambda e: e.activation(out=xtok[:], in_=b1[:], func=AF.Copy), r=["b1"], w=["xtok"])
                c.op("dve", lambda e: e.tensor_copy(Btok[:].rearrange("p a b -> p (a b)"), b2[:, 0:256]), r=["b2"], w=["Btok"])
                c.op("dve", lambda e: e.tensor_tensor(out=xc[:].rearrange("p (a b) -> p a b", a=16), in0=xtok[:].rearrange("p (a b) -> p a b", a=16),
                                                       in1=dtv[:, i, :].unsqueeze(2).to_broadcast([128, 16, 64]), op=ALU.mult), r=["xtok", "dtv"], w=["xc"])
                c.op("pool", lambda e: e.tensor_tensor(out=xcd[:].rearrange("p (a b) -> p a b", a=16), in0=xc[:].rearrange("p (a b) -> p a b", a=16),
                                                        in1=dec[:, 0:16].unsqueeze(2).to_broadcast([128, 16, 64]), op=ALU.mult), r=["xc", "dec"], w=["xcd"])
                if own:
                    row0 = oblk * 512 + i * 128
                    c.dma("sp", zs[:], S["z"][row0:row0 + 128, :], r=["z_d"], w=["zs"], stream="zs")
                    for g in range(2):
                        c.op("pe", lambda e: e.matmul(b0[:, 128 + g * 128:256 + g * 128], xbcT[:, 8 + g, ts], xbcT[:, 10 + g, ts], start=True, stop=True), r=["xbcT", "b0"], w=["b0"])
                    c.op("dve", lambda e: e.tensor_tensor(out=cbm[:], in0=b0[:, 128:384].rearrange("p (a b) -> p a b", a=2), in1=P["tri"][:].unsqueeze(1).to_broadcast([128, 2, 128]), op=ALU.mult),
                         r=["b0", "tri"], w=["cbm"])
                    c.op("pool", lambda e: e.tensor_tensor(out=diag[:], in0=P["identf"][:].unsqueeze(1).to_broadcast([128, 16, 128]), in1=acs[:, 0:16].unsqueeze(2).to_broadcast([128, 16, 128]), op=ALU.mult),
                         r=["identf", "acs"], w=["diag"])
                    for k4 in range(4):
                        pb = b34[k4 % 2]; pk = "b%d" % (3 + k4 % 2)
                        c.op("pe", lambda e: e.matmul(pb[:], P["ones"][:], diag[:, k4 * 4:(k4 + 1) * 4, :].rearrange("p a b -> p (a b)"), start=True, stop=True), r=["ones", "diag"], w=[pk])
                        for jj in range(4):
                            j = k4 * 4 + jj
                            c.op("dve", lambda e: e.tensor_scalar(out=seg[:, j, :], in0=pb[:, jj * 128:(jj + 1) * 128], scalar1=acs[:, j:j + 1], scalar2=0.0, op0=ALU.subtract, op1=ALU.min),
                                 r=[pk, "acs", "seg"], w=["seg"])
                    c.op("act", lambda e: e.activation(out=seg[:].rearrange("p a b -> p (a b)"), in_=seg[:].rearrange("p a b -> p (a b)"), func=AF.Exp), r=["seg"], w=["seg"])
                    for g in range(2):
                        c.op("dve", lambda e: e.tensor_tensor(out=Mt[:, 8 * g:8 * g + 8, :], in0=seg[:, 8 * g:8 * g + 8, :], in1=cbm[:, g, :].unsqueeze(1).to_broadcast([128, 8, 128]), op=ALU.mult),
                             r=["seg", "cbm", "Mt"], w=["Mt"])
                    for hh in range(16):
                        pb = b56[hh // 8]; pk = "b%d" % (5 + hh // 8)
                        c.op("pe", lambda e: e.matmul(pb[:, (hh % 8) * 64:(hh % 8 + 1) * 64], Mt[:, hh, :], xc[:, hh * 64:(hh + 1) * 64], start=True, stop=True), r=["Mt", "xc", pk], w=[pk])
                    for g in range(2):
                        gs = slice(g * 512, (g + 1) * 512)
                        c.op("pe", lambda e: e.matmul(b7[:], xbcT[:, 10 + g, ts], state_b[:, g, :], start=True, stop=True), r=["xbcT", "state_b"], w=["b7"])
                        c.op("dve", lambda e: e.tensor_tensor(out=t1[:, gs].rearrange("p (a b) -> p a b", a=8), in0=b7[:].rearrange("p (a b) -> p a b", a=8),
                                                               in1=dec[:, 32 + 8 * g:40 + 8 * g].unsqueeze(2).to_broadcast([128, 8, 64]), op=ALU.mult), r=["b7", "dec", "t1"], w=["t1"])
                        c.op("dve", lambda e: e.tensor_tensor(out=t1[:, gs], in0=t1[:, gs], in1=b56[g][:], op=ALU.add), r=["t1", "b%d" % (5 + g)], w=["t1"])
                    c.op("pool", lambda e: e.tensor_tensor(out=t2[:].rearrange("p (a b) -> p a b", a=16), in0=xtok[:].rearrange("p (a b) -> p a b", a=16),
                                                            in1=P["dsk"][:].unsqueeze(2).to_broadcast([128, 16, 64]), op=ALU.mult), r=["xtok", "dsk"], w=["t2"])
                    c.op("dve", lambda e: e.tensor_tensor(out=t1[:], in0=t1[:], in1=t2[:], op=ALU.add), r=["t1", "t2"], w=["t1"])
                    c.op("dve", lambda e: e.tensor_tensor(out=t1[:], in0=t1[:], in1=zs[:], op=ALU.mult), r=["t1", "zs"], w=["t1"])
                    for g in range(2):
                        c.op("act", lambda e: e.activation(out=junk2[:], in_=t1[:, g * 512:(g + 1) * 512], func=AF.Square, accum_out=ssg[:, g:g + 1]), r=["t1", "ssg"], w=["junk2", "ssg"])
                    c.op("dve", lambda e: e.tensor_scalar(out=ssg[:], in0=ssg[:], scalar1=1.0 / 512, scalar2=EPS, op0=ALU.mult, op1=ALU.add), r=["ssg"], w=["ssg"])
                    c.op("act", lambda e: e.activation(out=ssg[:], in_=ssg[:], func=AF.Sqrt), r=["ssg"], w=["ssg"])
                    c.op("dve", lambda e: e.reciprocal(out=ssg[:], in_=ssg[:]), r=["ssg"], w=["ssg"])
                    for g in range(2):
                        gs = slice(g * 512, (g + 1) * 512)
                        c.op("dve", lambda e: e.scalar_tensor_tensor(out=yo[:, gs], in0=t1[:, gs], scalar=ssg[:, g:g + 1], in1=P["ssmnw"][:, gs], op0=ALU.mult, op1=ALU.mult),
                             r=["t1", "ssg", "ssmnw", "yo"], w=["yo"])
                    c.dma("pool", S["mix"][row0:row0 + 128, 1024:2048], yo[:], r=["yo"], w=["mix_d"], stream="yo")
                for g in range(2):
                    pb = b34[g]; pk = "b%d" % (3 + g)
                    c.op("pe", lambda e: e.matmul(pb[:], Btok[:, g, :], xcd[:, g * 512:(g + 1) * 512], start=True, stop=True), r=["Btok", "xcd"], w=[pk])
                    c.op("dve", lambda e: e.tensor_tensor(out=state_f[:, g, :].rearrange("p (a b) -> p a b", a=8), in0=state_f[:, g, :].rearrange("p (a b) -> p a b", a=8),
                                                           in1=dec[:, 16 + 8 * g:24 + 8 * g].unsqueeze(2).to_broadcast([128, 8, 64]), op=ALU.mult), r=["state_f", "dec"], w=["state_f"])
                    c.op("dve", lambda e: e.tensor_tensor(out=state_f[:, g, :], in0=state_f[:, g, :], in1=pb[:], op=ALU.add), r=["state_f", pk], w=["state_f"])
                c.op("act", lambda e: e.activation(out=state_b[:].rearrange("p a b -> p (a b)"), in_=state_f[:].rearrange("p a b -> p (a b)"), func=AF.Copy), r=["state_f"], w=["state_b"])
        c.barrier()


def phase_attn(c, cfg, I, S, P):
    T0 = cfg.T0; QB = cfg.OWNBLK
    with contextlib.ExitStack() as pes:
        kT = c.sb("kT", [128, cfg.SLOC], BF16, pes); v = c.sb("v", [128, cfg.NT, 129], BF16, pes); qz = [c.sb("qz%d" % i, [128, cfg.QTR], BF16, pes) for i in range(2)]
        pT = [c.sb("pT%d" % i, [128, 2, 512], BF16, pes) for i in range(2)]
        tmpb = [c.sb("tmpb%d" % i, [128, 128], F32, pes) for i in range(2)]
        pn = [c.sb("pn%d" % i, [128, 128], BF16, pes) for i in range(2)]
        pslb = [c.ps("pslb%d" % i, [128, 1024], F32, pes) for i in range(2)]
        psn = c.ps("psn", [128, 512], F32, pes)
        pacc = [c.ps("pacc%d" % i, [128, 512], F32, pes) for i in range(3)]
        r12 = c.sb("r12", [128, 2], F32, pes); nl = c.sb("nl", [128, 1], F32, pes)
        o1 = c.sb("o1", [128, 128], F32, pes); dd = c.sb("dd", [128, 128], F32, pes); junk3 = c.sb("junk3", [128, 128], BF16, pes)
        ssa = c.sb("ssa", [128, 1], F32, pes)
        atto = c.sb("atto", [128, 4, 128], BF16, pes)
        def acc(m, a):
            idx = m * 4 + a
            return pacc[idx // 3][:, (idx % 3) * 129:(idx % 3) * 129 + 129], "pacc%d" % (idx // 3)
        nn = 0
        c.op("dve", lambda e: e.memset(qz[0][64:128, :], 0.0), w=["qz0"])
        c.op("dve", lambda e: e.memset(qz[1][0:64, :], 0.0), w=["qz1"])
        for h in range(H):
            c.dma("sp", kT[:], S["kT"][h], r=["kT_d"], w=["kT"], stream="kT")
            c.dma("sp", v[:], S["v"][h], r=["v_d"], w=["v"], stream="v")
            c.dma("sp", qz[0][0:64, :], S["qT"][h, 0:64, :], r=["qT_d"], w=["qz0"], stream="qz0")
            c.dma("sp", qz[1][64:128, :], S["qT"][h, 64:128, :], r=["qT_d"], w=["qz1"], stream="qz1")
            for qb in range(QB):
                nfar = T0 + 4 * qb - 1
                started = set()
                def L_(kt):
                    pb = kt % 2
                    for m in range(2):
                        ms = slice(m * 64, (m + 1) * 64)
                        c.op("pe", lambda e: e.matmul(pslb[pb][:, m * 512:(m + 1) * 512], kT[:, kt * 128:(kt + 1) * 128], qz[m][:, qb * 512:(qb + 1) * 512], start=True, stop=True),
                             r=["kT", "qz%d" % m, "pslb%d" % pb], w=["pslb%d" % pb])
                def X_(kt):
                    pb = kt % 2
                    c.op("act", lambda e: e.activation(out=pT[pb][:].rearrange("p a b -> p (a b)"), in_=pslb[pb][:], func=AF.Exp),
                         r=["pslb%d" % pb], w=["pT%d_0" % pb, "pT%d_1" % pb])
                def PV_(kt):
                    pb = kt % 2
                    for m in range(2):
                        for a in range(4):
                            ap_, ak = acc(m, a)
                            c.op("pe", lambda e: e.matmul(ap_, pT[pb][:, m, a * 128:(a + 1) * 128], v[:, kt, :], start=((m, a) not in started), stop=False),
                                 r=["pT%d_%d" % (pb, m), "v", ak], w=[ak])
                            started.add((m, a))
                L_(0)
                for kt in range(nfar):
                    if kt + 1 < nfar: L_(kt + 1)
                    X_(kt)
                    PV_(kt)
                for a in range(4):
                    qt = T0 + 4 * qb + a
                    for kt in range(T0 + 4 * qb - 1, qt + 1):
                        diff = qt - kt
                        for m in range(2):
                            ms = slice(m * 64, (m + 1) * 64)
                            sl = nn % 4; bi = nn % 2; nn += 1
                            pv = psn[:, sl * 128:(sl + 1) * 128]
                            c.op("pe", lambda e: e.matmul(pv, kT[:, kt * 128:(kt + 1) * 128], qz[m][:, qb * 512 + a * 128:qb * 512 + (a + 1) * 128], start=True, stop=True),
                                 r=["kT", "qz%d" % m], w=["psn%d" % sl])
                            if diff <= 1:
                                bt = P["bdiag"] if diff == 0 else P["bprev"]
                                c.op("dve", lambda e: e.tensor_tensor(out=tmpb[bi][:], in0=pv, in1=bt[:, h * 2 + m, :], op=ALU.add), r=["psn%d" % sl, "bdiag", "bprev"], w=["tmpb%d" % bi])
                                c.op("act", lambda e: e.activation(out=pn[bi][:], in_=tmpb[bi][:], func=AF.Exp), r=["tmpb%d" % bi], w=["pn%d" % bi])
                            else:
                                c.op("act", lambda e: e.activation(out=pn[bi][:], in_=pv, func=AF.Exp), r=["psn%d" % sl], w=["pn%d" % bi])
                            ap_, ak = acc(m, a)
                            c.op("pe", lambda e: e.matmul(ap_, pn[bi][:], v[:, kt, :], start=((m, a) not in started), stop=(diff == 0)), r=["pn%d" % bi, "v", ak], w=[ak])
                            started.add((m, a))
                for a in range(4):
                    a1, k1 = acc(0, a); a2, k2 = acc(1, a)
                    c.op("dve", lambda e: e.reciprocal(out=r12[:, 0:1], in_=a1[:, 128:129]), r=[k1, "r12"], w=["r12"])
                    c.op("dve", lambda e: e.reciprocal(out=r12[:, 1:2], in_=a2[:, 128:129]), r=[k2, "r12"], w=["r12"])
                    c.op("dve", lambda e: e.tensor_tensor(out=nl[:], in0=r12[:, 1:2], in1=P["neglam"][:], op=ALU.mult), r=["r12", "neglam"], w=["nl"])
                    c.op("dve", lambda e: e.tensor_scalar(out=o1[:], in0=a1[:, 0:128], scalar1=r12[:, 0:1], scalar2=None, op0=ALU.mult), r=[k1, "r12"], w=["o1"])
                    c.op("dve", lambda e: e.scalar_tensor_tensor(out=dd[:], in0=a2[:, 0:128], scalar=nl[:, 0:1], in1=o1[:], op0=ALU.mult, op1=ALU.add), r=[k2, "nl", "o1"], w=["dd"])
                    c.op("act", lambda e: e.activation(out=junk3[:], in_=dd[:], func=AF.Square, accum_out=ssa[:, 0:1]), r=["dd"], w=["junk3", "ssa"])
                    c.op("dve", lambda e: e.tensor_scalar(out=ssa[:], in0=ssa[:], scalar1=1.0 / 128, scalar2=EPS, op0=ALU.mult, op1=ALU.add), r=["ssa"], w=["ssa"])
                    c.op("act", lambda e: e.activation(out=ssa[:], in_=ssa[:], func=AF.Sqrt), r=["ssa"], w=["ssa"])
                    c.op("dve", lambda e: e.reciprocal(out=ssa[:], in_=ssa[:]), r=["ssa"], w=["ssa"])
                    c.op("dve", lambda e: e.scalar_tensor_tensor(out=atto[:, a, :], in0=dd[:], scalar=ssa[:, 0:1], in1=P["subln"][:], op0=ALU.mult, op1=ALU.mult), r=["dd", "ssa", "subln", "atto"], w=["atto"])
                c.dma("pool", S["mix"][qb * 512:(qb + 1) * 512, h * 128:(h + 1) * 128].rearrange("(n p) c -> p n c", p=128), atto[:], r=["atto"], w=["mix_d"], stream="atto")
        c.barrier()


def phase_out(c, cfg, I, S, P, y_out):
    xown0 = cfg.SLOC - cfg.QTR
    with contextlib.ExitStack() as pes:
        wo = c.sb("wo", [128, 16, D], BF16, pes)
        c.dma("sp", wo[:], S["w_out_bf"].rearrange("(c p) n -> p c n", p=128), r=["w_out_bf"], w=["wo"], stream="wo")
        mx = [c.sb("mx%d" % i, [128, D], BF16, pes) for i in range(2)]
        mixT = c.sb("mixT", [128, 16, 512], BF16, pes)
        xt = [c.sb("xta%d" % i, [128, D], F32, pes) for i in range(2)]
        ht = [c.sb("ht%d" % i, [128, D], F32, pes) for i in range(2)]
        ptr = [c.ps("ptr%d" % i, [128, 1024], BF16, pes) for i in range(2)]
        pso = [c.ps("pso%d" % i, [128, 512], F32, pes) for i in range(4)]
        for ob in range(cfg.OWNBLK):
            for i in range(4):
                bi = i % 2
                r0 = ob * 512 + i * 128
                c.dma("sp", mx[bi][:], S["mix"][r0:r0 + 128, :], r=["mix_d"], w=["mx%d" % bi], stream="mx%d" % bi)
                for half in range(2):
                    for j in range(8):
                        ch = half * 8 + j
                        c.op("pe", lambda e: e.transpose(ptr[half][:, j * 128:(j + 1) * 128], mx[bi][:, ch * 128:(ch + 1) * 128], P["ident"][:]), r=["mx%d" % bi, "ident"], w=["ptr%d" % half])
                    eng = "act" if half == 0 else "dve"
                    if eng == "act":
                        c.op("act", lambda e: e.activation(out=mixT[:, half * 8:half * 8 + 8, i * 128:(i + 1) * 128], in_=ptr[half][:].rearrange("p (a b) -> p a b", a=8), func=AF.Copy), r=["ptr%d" % half, "mixT"], w=["mixT"])
                    else:
                        c.op("dve", lambda e: e.tensor_copy(mixT[:, half * 8:half * 8 + 8, i * 128:(i + 1) * 128], ptr[half][:].rearrange("p (a b) -> p a b", a=8)), r=["ptr%d" % half, "mixT"], w=["mixT"])
            for i in range(4):
                bi = i % 2
                r0 = ob * 512 + i * 128
                c.dma("sp", xt[bi][:], I["xloc"][xown0 + r0:xown0 + r0 + 128, :], w=["xta%d" % bi], stream="xta%d" % bi)
                for db in range(4):
                    for kc in range(16):
                        c.op("pe", lambda e: e.matmul(pso[db][:], mixT[:, kc, i * 128:(i + 1) * 128], wo[:, kc, db * 512:(db + 1) * 512], start=(kc == 0), stop=(kc == 15)), r=["mixT", "wo"], w=["pso%d" % db])
                    ds_ = slice(db * 512, (db + 1) * 512)
                    c.op("dve", lambda e: e.tensor_tensor(out=ht[bi][:, ds_], in0=pso[db][:], in1=P["g1b"][:, ds_], op=ALU.mult), r=["pso%d" % db, "g1b", "ht%d" % bi], w=["ht%d" % bi])
                    c.op("pool", lambda e: e.tensor_tensor(out=ht[bi][:, ds_], in0=ht[bi][:, ds_], in1=xt[bi][:, ds_], op=ALU.add), r=["ht%d" % bi, "xta%d" % bi], w=["ht%d" % bi])
                c.dma("pool", S["h"][r0:r0 + 128, :], ht[bi][:], r=["ht%d" % bi], w=["h_d"], stream="ht%d" % bi)
        c.barrier()
    with contextlib.ExitStack() as pes:
        hn2T = c.sb("hn2T", [128, 16, 512], BF16, pes)
        for ob in range(cfg.OWNBLK):
            with contextlib.ExitStack() as aes:
                qpT = c.sb("qpT", [128, 16, 512], BF16, aes)
                with contextlib.ExitStack() as a1:
                    xt = [c.sb("xtb%d" % i, [128, D], F32, a1) for i in range(2)]
                    junk = c.sb("junkb", [128, D], BF16, a1); xs = c.sb("xsb", [128, 4, D], BF16, a1); ssq = c.sb("ssqb", [128, 4], F32, a1)
                    pst = [c.ps("pstb%d" % i, [128, 1024], BF16, a1) for i in range(2)]
                    norm_transpose(c, (xt, junk, xs, ssq, pst, P), S["h"][ob * 512:(ob + 1) * 512, :], P["w2p"], P["sh2T"], "wp48", "sh48", hn2T, "hn2T", "b")
                    c.barrier()
                with contextlib.ExitStack() as a2:
                    wg = [c.sb("wqg%d" % i, [128, 16, 512], BF16, a2) for i in range(2)]
                    psq = [c.ps("psq%d" % i, [128, 512], F32, a2) for i in range(2)]
                    wv = S["wq_bf"].rearrange("(c p) n -> p c n", p=128)
                    for g in range(4):
                        c.dma("sp", wg[g % 2][:], wv[:, :, g * 512:(g + 1) * 512], r=["wq_bf"], w=["wqg%d" % (g % 2)], stream="wqg%d" % (g % 2))
                        for cc in range(4):
                            ch = g * 4 + cc; pb = ch % 2
                            for kc in range(16):
                                c.op("pe", lambda e: e.matmul(psq[pb][:], wg[g % 2][:, kc, cc * 128:(cc + 1) * 128], hn2T[:, kc, :], start=(kc == 0), stop=(kc == 15)), r=["wqg%d" % (g % 2), "hn2T"], w=["psq%d" % pb])
                            c.op("act", lambda e: e.activation(out=qpT[:, ch, :], in_=psq[pb][:], func=AF.Copy), r=["psq%d" % pb, "qpT"], w=["qpT"])
                    c.barrier()
                with contextlib.ExitStack() as a3:
                    Ssb = c.sb("Ssb", [128, 16, 128], F32, a3); mxs = c.sb("mxs", [128, 16], F32, a3)
                    T16 = c.sb("T16", [128, 16, 16], F32, a3); tmpk = c.sb("tmpk", [128, 256], F32, a3)
                    Pc = c.sb("Pc", [128, 8, 256], F32, a3); c8 = c.sb("c8", [128, 8, 16], F32, a3)
                    zz = c.sb("zz", [128, 8], F32, a3)
                    tmpP = [c.sb("tmpP%d" % i, [128, 32, 128], F32, a3) for i in range(2)]; tmpPb = [c.sb("tmpPb%d" % i, [128, 32 * 128], BF16, a3) for i in range(2)]; Gcb = [c.sb("Gcb%d" % i, [128, 32 * 128], BF16, a3) for i in range(2)]
                    pss = [c.ps("pss%d" % i, [128, 512], F32, a3) for i in range(4)]
                    gn = 0
                    for i in range(4):
                        for ch in range(16):
                            c.op("pe", lambda e: e.matmul(pss[ch // 4][:, (ch % 4) * 128:(ch % 4 + 1) * 128], qpT[:, ch, i * 128:(i + 1) * 128], P["keysT"][:, ch, :], start=True, stop=True),
                                 r=["qpT", "keysT", "pss%d" % (ch // 4)], w=["pss%d" % (ch // 4)])
                        for q4 in range(4):
                            c.op("dve", lambda e: e.tensor_copy(Ssb[:, q4 * 4:(q4 + 1) * 4, :].rearrange("p a b -> p (a b)"), pss[q4][:]), r=["pss%d" % q4, "Ssb"], w=["Ssb"])
                        c.op("dve", lambda e: e.tensor_reduce(out=mxs[:], in_=Ssb[:], axis=AX.X, op=ALU.max), r=["Ssb"], w=["mxs"])
                        c.op("dve", lambda e: e.tensor_tensor(out=Ssb[:], in0=Ssb[:], in1=mxs[:].unsqueeze(2).to_broadcast([128, 16, 128]), op=ALU.subtract), r=["Ssb", "mxs"], w=["Ssb"])
                        c.op("act", lambda e: e.activation(out=Ssb[:].rearrange("p a b -> p (a b)"), in_=Ssb[:].rearrange("p a b -> p (a b)"), func=AF.Exp), r=["Ssb"], w=["Ssb"])
                        for ch in range(16):
                            c.op("dve", lambda e: e.max(out=T16[:, ch, 0:8], in_=Ssb[:, ch, :]), r=["Ssb", "T16"], w=["T16"])
                            c.op("dve", lambda e: e.match_replace(out=tmpk[:, 0:128], in_to_replace=T16[:, ch, 0:8], in_values=Ssb[:, ch, :], imm_value=-1.0), r=["Ssb", "T16"], w=["tmpk"])
                            c.op("dve", lambda e: e.max(out=T16[:, ch, 8:16], in_=tmpk[:, 0:128]), r=["tmpk", "T16"], w=["T16"])
                        T4 = T16[:].rearrange("p (h c) k -> p h c k", c=2)
                        c.op("dve", lambda e: e.tensor_tensor(out=Pc[:].rearrange("p h (a b) -> p h a b", a=16), in0=T4[:, :, 0, :].unsqueeze(3).to_broadcast([128, 8, 16, 16]),
                                                               in1=T4[:, :, 1, :].unsqueeze(2).to_broadcast([128, 8, 16, 16]), op=ALU.mult), r=["T16"], w=["Pc"])
                        for hh in range(8):
                            c.op("dve", lambda e: e.max(out=c8[:, hh, 0:8], in_=Pc[:, hh, :]), r=["Pc", "c8"], w=["c8"])
                            c.op("dve", lambda e: e.match_replace(out=tmpk[:], in_to_replace=c8[:, hh, 0:8], in_values=Pc[:, hh, :], imm_value=-1.0), r=["Pc", "c8"], w=["tmpk"])
                            c.op("dve", lambda e: e.max(out=c8[:, hh, 8:16], in_=tmpk[:]), r=["tmpk", "c8"], w=["c8"])
                        c.op("dve", lambda e: e.tensor_reduce(out=zz[:], in_=c8[:], axis=AX.X, op=ALU.add), r=["c8"], w=["zz"])
                        c.op("dve", lambda e: e.reciprocal(out=zz[:], in_=zz[:]), r=["zz"], w=["zz"])
                        for q in range(4):
                            gb = gn % 2; gn += 1
                            for hh in range(8):
                                tp = tmpP[hh % 2]; tk = "tmpP%d" % (hh % 2)
                                if hh % 2 == 0:
                                    c.op("pool", lambda e: e.tensor_tensor(out=tp[:], in0=Ssb[:, 2 * hh, q * 32:(q + 1) * 32].unsqueeze(2).to_broadcast([128, 32, 128]),
                                                                            in1=Ssb[:, 2 * hh + 1, :].unsqueeze(1).to_broadcast([128, 32, 128]), op=ALU.mult), r=["Ssb"], w=[tk])
                                else:
                                    for i1 in range(32):
                                        c.op("act", lambda e: e.activation(out=tp[:, i1, :], in_=Ssb[:, 2 * hh + 1, :], func=AF.Copy, scale=Ssb[:, 2 * hh, q * 32 + i1:q * 32 + i1 + 1]), r=["Ssb", tk], w=[tk])
                                tf = tp[:].rearrange("p a b -> p (a b)")
                                tb = tmpPb[hh % 2]; tbk = "tmpPb%d" % (hh % 2)
                                c.op("dve", lambda e: e.scalar_tensor_tensor(out=tb[:], in0=tf, scalar=c8[:, hh, 15:16], in1=tf, op0=ALU.is_ge, op1=ALU.mult), r=[tk, "c8"], w=[tbk])
                                if hh == 0:
                                    c.op("dve", lambda e: e.tensor_scalar(out=Gcb[gb][:], in0=tb[:], scalar1=zz[:, hh:hh + 1], scalar2=None, op0=ALU.mult), r=[tbk, "zz"], w=["Gcb%d" % gb])
                                else:
                                    c.op("dve", lambda e: e.scalar_tensor_tensor(out=Gcb[gb][:], in0=tb[:], scalar=zz[:, hh:hh + 1], in1=Gcb[gb][:], op0=ALU.mult, op1=ALU.add), r=[tbk, "zz", "Gcb%d" % gb], w=["Gcb%d" % gb])
                            r0 = ob * 512 + i * 128
                            c.dma("sp", S["G"][r0:r0 + 128, q * 4096:(q + 1) * 4096], Gcb[gb][:], r=["Gcb%d" % gb], w=["G_d"], stream="Gcb%d" % gb)
                    c.barrier()
            with contextlib.ExitStack() as bes:
                dn = [c.sb("dn%d" % i, [128, 16, 512], BF16, bes) for i in range(2)]
                upb = [c.sb("upb%d" % i, [128, 4, D], BF16, bes) for i in range(2)]
                gsb = [c.sb("gsb%d" % i, [128, 4, 512], BF16, bes) for i in range(2)]
                oacc = c.sb("oacc", [128, 4, D], F32, bes)
                gl = [c.sb("gl%d" % i, [128, 512], F32, bes) for i in range(2)]
                at = [c.sb("at%d" % i, [128, 512], BF16, bes) for i in range(2)]
                atT = [c.sb("atT%d" % i, [128, 4, 128], BF16, bes) for i in range(2)]
                hb = c.sb("hb", [128, D], F32, bes)
                ppre = [c.ps("ppre%d" % i, [128, 512], F32, bes) for i in range(2)]
                ptr = c.ps("ptrb", [128, 1024], BF16, bes)
                po = [c.ps("po%d" % i, [128, 512], F32, bes) for i in range(4)]
                dv = S["downT_bf"].rearrange("(c p) n -> p c n", p=128)
                NEB = NEXP // 512
                items = [(eb, i) for eb in range(NEB) for i in range(4)]
                def LOADW(eb):
                    wb = eb % 2
                    c.dma("sp", dn[wb][:], dv[:, :, eb * 512:(eb + 1) * 512], r=["downT_bf"], w=["dn%d" % wb], stream="dn%d" % wb)
                    c.dma("sp", upb[wb][:], S["up_bf"][eb * 512:(eb + 1) * 512, :].rearrange("(c p) n -> p c n", p=128), r=["up_bf"], w=["upb%d" % wb], stream="upb%d" % wb)
                    c.dma("pool", gsb[wb][:], S["G"][ob * 512:(ob + 1) * 512, eb * 512:(eb + 1) * 512].rearrange("(n p) c -> p n c", p=128), r=["G_d"], w=["gsb%d" % wb], stream="gsb%d" % wb)
                def PRE(n):
                    eb, i = items[n]; wb = eb % 2; bi = n % 2
                    if i == 0: LOADW(eb)
                    for kc in range(16):
                        c.op("pe", lambda e: e.matmul(ppre[bi][:], hn2T[:, kc, i * 128:(i + 1) * 128], dn[wb][:, kc, :], start=(kc == 0), stop=(kc == 15)), r=["hn2T", "dn%d" % wb], w=["ppre%d" % bi])
                def ACTS(n):
                    eb, i = items[n]; wb = eb % 2; bi = n % 2
                    c.op("act", lambda e: e.activation(out=gl[bi][:], in_=ppre[bi][:], func=AF.Gelu), r=["ppre%d" % bi], w=["gl%d" % bi])
                    c.op("dve", lambda e: e.tensor_tensor(out=at[bi][:], in0=gl[bi][:], in1=gsb[wb][:, i, :], op=ALU.mult), r=["gl%d" % bi, "gsb%d" % wb], w=["at%d" % bi])
                def TRUP(n):
                    eb, i = items[n]; wb = eb % 2; bi = n % 2
                    for ec in range(4):
                        c.op("pe", lambda e: e.transpose(ptr[:, bi * 512 + ec * 128:bi * 512 + (ec + 1) * 128], at[bi][:, ec * 128:(ec + 1) * 128], P["ident"][:]), r=["at%d" % bi, "ident"], w=["ptr%d" % bi])
                    c.op("act", lambda e: e.activation(out=atT[bi][:].rearrange("p a b -> p (a b)"), in_=ptr[:, bi * 512:(bi + 1) * 512], func=AF.Copy), r=["ptr%d" % bi], w=["atT%d" % bi])
                def UP(n):
                    eb, i = items[n]; wb = eb % 2; bi = n % 2
                    for db in range(4):
                        for ec in range(4):
                            c.op("pe", lambda e: e.matmul(po[db][:], atT[bi][:, ec, :], upb[wb][:, ec, db * 512:(db + 1) * 512], start=(ec == 0), stop=(ec == 3)), r=["atT%d" % bi, "upb%d" % wb], w=["po%d" % db])
                        ds_ = slice(db * 512, (db + 1) * 512)
                        if eb == 0:
                            c.op("dve", lambda e: e.tensor_copy(oacc[:, i, ds_], po[db][:]), r=["po%d" % db, "oacc%d" % i], w=["oacc%d" % i])
                        else:
                            c.op("dve", lambda e: e.tensor_tensor(out=oacc[:, i, ds_], in0=oacc[:, i, ds_], in1=po[db][:], op=ALU.add), r=["po%d" % db, "oacc%d" % i], w=["oacc%d" % i])
                PRE(0)
                for n in range(len(items)):
                    if n + 1 < len(items): PRE(n + 1)
                    ACTS(n)
                    TRUP(n)
                    if n >= 1: UP(n - 1)
                UP(len(items) - 1)
                for i in range(4):
                    r0 = ob * 512 + i * 128
                    c.dma("sp", hb[:], S["h"][r0:r0 + 128, :], r=["h_d"], w=["hb"], stream="hb")
                    c.op("dve", lambda e: e.tensor_tensor(out=oacc[:, i, :], in0=oacc[:, i, :], in1=P["g2b"][:], op=ALU.mult), r=["oacc%d" % i, "g2b"], w=["oacc%d" % i])
                    c.op("dve", lambda e: e.tensor_tensor(out=oacc[:, i, :], in0=oacc[:, i, :], in1=hb[:], op=ALU.add), r=["oacc%d" % i, "hb"], w=["oacc%d" % i])
                    c.dma("pool", y_out[r0:r0 + 128, :], oacc[:, i, :], r=["oacc%d" % i], w=["y"], stream="yout")
                c.barrier()
        c.barrier()


_NC_CACHE = {}

def kernel(**inputs):
    cfg = Cfg(int(np.asarray(inputs["x"]).shape[1]))
    if cfg.SEQ not in _NC_CACHE:
        _NC_CACHE[cfg.SEQ] = build(cfg)
    nc = _NC_CACHE[cfg.SEQ]
    maps = prep_inputs(inputs, cfg)
    res = run_bass_kernel_spmd(nc, maps, core_ids=list(range(8)))
    B = np.asarray(inputs["x"]).shape[0]
    out = np.zeros((B, cfg.SEQ, D), np.float32)
    for core in range(8):
        b, j = core // 4, core % 4
        out[b, j * cfg.QTR:(j + 1) * cfg.QTR] = np.asarray(res.results[core]["y"])
    return out
```

```python
import contextlib, math
import numpy as np
import concourse.bass as bass
import concourse.mybir as mybir
from concourse.bass_utils import run_bass_kernel_spmd

F32 = mybir.dt.float32; BF16 = mybir.dt.bfloat16
AF = mybir.ActivationFunctionType; ALU = mybir.AluOpType; AX = mybir.AxisListType

D = 2048; NCH = 16; H = 8; NEXP = 16384
EPS = 1e-6
LAM_INIT = 0.2
SAME_ENGINE_SYNC = True
ATTACH_WAIT = True

class _Eng:
    def __init__(self, name, eng, sem):
        self.name = name; self.eng = eng; self.sem = sem; self.count = 0; self.seen = {}

class _Stream:
    def __init__(self, sem):
        self.sem = sem; self.count = 0

class Ctx:
    def __init__(self, nc, es):
        self.nc = nc; self.es = es; self.E = {}
        for name, e in [("pe", nc.tensor), ("act", nc.scalar), ("dve", nc.vector), ("pool", nc.gpsimd), ("sp", nc.sync)]:
            self.E[name] = _Eng(name, e, es.enter_context(nc.semaphore("sem_" + name)))
        self.lastw = {}; self.reads = {}; self.streams = {}; self.nins = 0
    def sb(self, name, shape, dt, es=None):
        self.nins += 0; self._uid = getattr(self, "_uid", 0) + 1
        return (es or self.es).enter_context(self.nc.sbuf_tensor("s%d_%s" % (self._uid, name), list(shape), dt))
    def ps(self, name, shape, dt, es=None):
        self._uid = getattr(self, "_uid", 0) + 1
        return (es or self.es).enter_context(self.nc.psum_tensor("p%d_%s" % (self._uid, name), list(shape), dt))
    def _deps(self, r, w):
        ev = []
        for k in r:
            if k in self.lastw: ev.append(self.lastw[k])
        for k in w:
            if k in self.lastw: ev.append(self.lastw[k])
            ev.extend(self.reads.get(k, []))
        return ev
    def _needed(self, E, ev):
        need = {}
        for (sem, v, owner) in ev:
            if owner is E and (not SAME_ENGINE_SYNC or E.name == "pe"):
                continue
            key = id(sem)
            if E.seen.get(key, 0) < v and (key not in need or need[key][1] < v):
                need[key] = (sem, v)
        return list(need.values())
    def _wait(self, E, ev, keep_last=False):
        need = self._needed(E, ev)
        last = need.pop() if (keep_last and need) else None
        for (sem, v) in need:
            E.eng.wait_ge(sem, v); E.seen[id(sem)] = v
        return last
    def _record(self, r, w, event):
        for k in r:
            self.reads.setdefault(k, []).append(event)
        for k in w:
            self.lastw[k] = event; self.reads[k] = []
    def op(self, en, fn, r=(), w=()):
        E = self.E[en]
        last = self._wait(E, self._deps(r, w), keep_last=ATTACH_WAIT)
        ins = fn(E.eng)
        if last is not None:
            ins._wait_ge(last[0], last[1]); E.seen[id(last[0])] = last[1]
        E.count += 1
        ins.then_inc(E.sem, 1)
        self._record(r, w, (E.sem, E.count, E))
        self.nins += 1
        return ins
    def dma(self, en, out, in_, r=(), w=(), stream=None, **kw):
        E = self.E[en]
        if stream not in self.streams:
            self.streams[stream] = _Stream(self.es.enter_context(self.nc.semaphore("ds_" + str(stream))))
        S = self.streams[stream]
        self._wait(E, self._deps(r, w))
        ins = E.eng.dma_start(out=out, in_=in_, **kw)
        S.count += 1
        ins.then_inc(S.sem, 16)
        self._record(r, w, (S.sem, 16 * S.count, S))
        self.nins += 1
        return ins
    def all_events(self):
        ev = list(self.lastw.values())
        for l in self.reads.values(): ev.extend(l)
        return ev
    def barrier(self):
        ev = self.all_events()
        best = {}
        for (sem, v, o) in ev:
            if id(sem) not in best or best[id(sem)][1] < v: best[id(sem)] = (sem, v, None)
        for E in self.E.values():
            self._wait(E, list(best.values()))
        self.lastw = {}; self.reads = {}

class Cfg:
    def __init__(self, seq=16384):
        self.SEQ = seq; self.QTR = seq // 4; self.SLOC = seq
        self.TB = 512; self.NBLK = self.SLOC // 512; self.OWNBLK = self.QTR // 512
        self.NT = self.SLOC // 128; self.T0 = 3 * self.QTR // 128

def _t5_bucket(rel):
    n = np.maximum(rel, 0)
    nf = np.maximum(n, 1).astype(np.float32)
    large = 16 + (np.log(nf / np.float32(16)) / np.float32(math.log(128 / 16)) * np.float32(16)).astype(np.int32)
    large = np.minimum(large, 31)
    return np.where(n < 16, n, large)

def _col16(v):
    return np.ascontiguousarray(np.asarray(v, np.float32).reshape(16, 128).T)

def prep_inputs(inp, cfg):
    f = lambda a: np.ascontiguousarray(np.asarray(a, np.float32))
    x = f(inp["x"]); B = x.shape[0]
    QTR, SLOC = cfg.QTR, cfg.SLOC
    shared = {}
    shared["ada_w"] = f(inp["ada_w"][0])
    shared["ada_bT"] = np.ascontiguousarray(f(inp["ada_b"][0]).reshape(96, 128).T)
    shared["ada_brow"] = f(inp["ada_b"][0]).reshape(1, 6 * D)
    shared["n1T"] = _col16(inp["norm1_w"][0]); shared["n2T"] = _col16(inp["norm2_w"][0])
    shared["w_in"] = f(inp["w_in"][0]); shared["w_out"] = f(inp["w_out"][0]); shared["peer_wq"] = f(inp["peer_wq"][0])
    shared["qnw"] = np.tile(f(inp["q_norm_w"][0]), 2).reshape(128, 1)
    shared["knw"] = np.tile(f(inp["k_norm_w"][0]), 2).reshape(128, 1)
    rb = f(inp["rel_bias"])
    shared["cb31"] = np.ascontiguousarray(np.broadcast_to(rb[31].reshape(1, 16), (128, 16)))
    kk = np.arange(128)[:, None]; qq = np.arange(128)[None, :]
    bd = rb[_t5_bucket(qq - kk)]
    bp = rb[_t5_bucket(qq - kk + 128)]
    shared["bdiag"] = np.ascontiguousarray(bd.reshape(128, 128, 16).transpose(0, 2, 1))
    shared["bprev"] = np.ascontiguousarray(bp.reshape(128, 128, 16).transpose(0, 2, 1))
    shared["cmask"] = np.where(qq >= kk, 0.0, -30000.0).astype(np.float32)
    shared["lam4"] = np.concatenate([f(inp["lambda_q1"][0]), f(inp["lambda_k1"][0]), f(inp["lambda_q2"][0]), f(inp["lambda_k2"][0])]).reshape(1, 256)
    shared["subln"] = f(inp["subln_w"][0]).reshape(1, 128)
    cw = f(inp["conv_w"][0])
    shared["convwT"] = np.ascontiguousarray(cw.T.reshape(12, 128, 4).transpose(1, 0, 2))
    shared["convb"] = np.ascontiguousarray(f(inp["conv_b"][0]).reshape(12, 128).T)
    shared["dtb"] = f(inp["dt_bias"][0]).reshape(1, 16); shared["alog"] = f(inp["a_log"][0]).reshape(1, 16)
    shared["dsk"] = f(inp["d_skip"][0]).reshape(1, 16)
    shared["ssmnw"] = f(inp["ssm_norm_w"][0]).reshape(1, 1024)
    pk = f(inp["peer_keys"][0]).reshape(16, 128, 128)
    shared["keysT"] = np.ascontiguousarray(pk.transpose(2, 0, 1))
    shared["downT"] = np.ascontiguousarray(f(inp["expert_down"][0]).T)
    shared["up"] = f(inp["expert_up"][0])
    shared["ident"] = np.eye(128, dtype=np.float32)
    shared["tri"] = (kk <= qq).astype(np.float32)
    bo = np.zeros((128, 128), np.float32); bo[:64, :64] = 1; bo[64:, 64:] = 1
    shared["blockones"] = bo
    maps = []
    for core in range(8):
        b, j = core // 4, core % 4
        m = dict(shared)
        xl = np.zeros((SLOC, D), np.float32)
        n = (j + 1) * QTR
        xl[SLOC - n:] = x[b, :n]
        m["xloc"] = xl
        vc = np.zeros((128, cfg.NBLK), np.float32); vc[:, cfg.NBLK - (j + 1) * cfg.OWNBLK:] = 1.0
        m["validcol"] = vc
        m["ccol"] = _col16(inp["c"][b])
        maps.append(m)
    return maps

IN_SHAPES = lambda cfg: {
    "xloc": [cfg.SLOC, D], "validcol": [128, cfg.NBLK], "ccol": [128, 16],
    "ada_w": [D, 6 * D], "ada_bT": [128, 96], "ada_brow": [1, 6 * D], "n1T": [128, 16], "n2T": [128, 16],
    "w_in": [D, 5648], "w_out": [D, D], "peer_wq": [D, D], "qnw": [128, 1], "knw": [128, 1],
    "cb31": [128, 16], "bdiag": [128, 16, 128], "bprev": [128, 16, 128], "cmask": [128, 128],
    "lam4": [1, 256], "subln": [1, 128], "convwT": [128, 12, 4], "convb": [128, 12],
    "dtb": [1, 16], "alog": [1, 16], "dsk": [1, 16], "ssmnw": [1, 1024], "keysT": [128, 16, 128],
    "downT": [D, NEXP], "up": [NEXP, D], "ident": [128, 128], "tri": [128, 128], "blockones": [128, 128],
}

def build(cfg, debug=False, stop_after=99):
    nc = bass.Bass("TRN2", target_bir_lowering=False)
    I = {k: nc.dram_tensor(k, s, F32, kind="ExternalInput").ap() for k, s in IN_SHAPES(cfg).items()}
    y_out = nc.dram_tensor("y", [cfg.QTR, D], F32, kind="ExternalOutput").ap()
    skind = "ExternalOutput" if debug else "Internal"
    def scratch(name, shape, dt):
        return nc.dram_tensor(name, list(shape), dt, kind=skind).ap()
    S = {}
    S["w_in_bf"] = scratch("w_in_bf", [D, 5648], BF16)
    S["w_out_bf"] = scratch("w_out_bf", [D, D], BF16)
    S["wq_bf"] = scratch("wq_bf", [D, D], BF16)
    S["downT_bf"] = scratch("downT_bf", [D, NEXP], BF16)
    S["up_bf"] = scratch("up_bf", [NEXP, D], BF16)
    S["kT"] = scratch("kT_d", [H, 128, cfg.SLOC], BF16)
    S["qT"] = scratch("qT_d", [H, 128, cfg.QTR], BF16)
    S["v"] = scratch("v_d", [H, 128, cfg.NT, 129], BF16)
    S["z"] = scratch("z_d", [cfg.QTR, 1024], BF16)
    S["uT"] = scratch("uT_d", [12, 128, cfg.SLOC], F32)
    S["dt"] = scratch("dt_d", [cfg.SLOC, 16], F32)
    S["mix"] = scratch("mix_d", [cfg.QTR, D], BF16)
    S["h"] = scratch("h_d", [cfg.QTR, D], F32)
    S["G"] = scratch("G_d", [cfg.QTR, NEXP], BF16)
    S["mod"] = scratch("mod_d", [128, 96], F32)
    es = contextlib.ExitStack()
    with es:
        c = Ctx(nc, es)
        P = {}
        phase0(c, cfg, I, S, P)
        if stop_after >= 1: phase_cast(c, cfg, I, S, P, stop_after)
        if stop_after >= 2: phase1a(c, cfg, I, S, P)
        if stop_after >= 3: phase1b(c, cfg, I, S, P)
        if stop_after >= 4: phase_attn(c, cfg, I, S, P)
        if stop_after >= 5: phase_out(c, cfg, I, S, P, y_out)
        if stop_after < 5:
            with contextlib.ExitStack() as pes:
                zt = c.sb("zt_dbg", [128, D], F32, pes)
                c.op("dve", lambda e: e.memset(zt[:], 0.0), w=["zt"])
                for i in range(cfg.QTR // 128):
                    c.dma("sp", y_out[i * 128:(i + 1) * 128, :], zt[:], r=["zt"], stream="yo")
                c.barrier()
        c.barrier()
    return nc


def phase0(c, cfg, I, S, P):
    es = c.es
    PERSIST = {"identf": [128, 128], "tri": [128, 128], "validcol": [128, cfg.NBLK], "qnw": [128, 1], "cb31": [128, 16],
               "bdiag": [128, 16, 128], "bprev": [128, 16, 128], "subln": [128, 128], "convwT": [128, 12, 4], "convb": [128, 12],
               "dtb": [128, 16], "dsk": [128, 16], "ssmnw": [128, 1024]}
    pre = {k: c.sb(k, sh, F32) for k, sh in PERSIST.items()}
    for k, sh, dt in (("ident", [128, 128], BF16), ("blockones", [128, 128], BF16), ("ones", [128, 128], F32), ("knw8", [128, 1], F32),
                      ("keysT", [128, 16, 128], BF16), ("aneg", [128, 16], F32), ("neglam", [128, 1], F32), ("g1b", [128, D], F32), ("g2b", [128, D], F32),
                      ("w1p", [128, 16], F32), ("sh1T", [128, 16], F32), ("w2p", [128, 16], F32), ("sh2T", [128, 16], F32)):
        pre[k] = c.sb(k, sh, dt)
    tes = contextlib.ExitStack()
    def load(name, shape, src, eng="sp", dt=F32):
        t = pre[name] if name in pre else c.sb(name, shape, dt, tes)
        c.dma(eng, t[:], src, w=[name], stream="ld_" + name)
        return t
    _sb0 = c.sb
    def _sb(name, shape, dt, es_=None):
        if name in pre: return pre[name]
        return _sb0(name, shape, dt, es_ or tes)
    c.sb = _sb
    P["identf"] = load("identf", [128, 128], I["ident"])
    P["tri"] = load("tri", [128, 128], I["tri"])
    bof = load("bof", [128, 128], I["blockones"])
    P["ident"] = c.sb("ident", [128, 128], BF16); P["blockones"] = c.sb("blockones", [128, 128], BF16)
    c.op("dve", lambda e: e.tensor_copy(P["ident"][:], P["identf"][:]), r=["identf"], w=["ident"])
    c.op("dve", lambda e: e.tensor_copy(P["blockones"][:], bof[:]), r=["bof"], w=["blockones"])
    P["ones"] = c.sb("ones", [128, 128], F32)
    c.op("dve", lambda e: e.memset(P["ones"][:], 1.0), w=["ones"])
    P["validcol"] = load("validcol", [128, cfg.NBLK], I["validcol"])
    ccol = load("ccol", [128, 16], I["ccol"])
    adabT = load("adabT", [128, 96], I["ada_bT"])
    n1T = load("n1T", [128, 16], I["n1T"]); n2T = load("n2T", [128, 16], I["n2T"])
    P["qnw"] = load("qnw", [128, 1], I["qnw"]); knw = load("knw", [128, 1], I["knw"])
    P["knw8"] = c.sb("knw8", [128, 1], F32)
    c.op("dve", lambda e: e.tensor_scalar(out=P["knw8"][:], in0=knw[:], scalar1=8.0, scalar2=None, op0=ALU.mult), r=["knw"], w=["knw8"])
    P["cb31"] = load("cb31", [128, 16], I["cb31"])
    P["bdiag"] = load("bdiag", [128, 16, 128], I["bdiag"]); P["bprev"] = load("bprev", [128, 16, 128], I["bprev"])
    cmask = load("cmask", [128, 128], I["cmask"])
    c.op("dve", lambda e: e.tensor_tensor(out=P["bdiag"][:], in0=P["bdiag"][:], in1=cmask[:].unsqueeze(1).to_broadcast([128, 16, 128]), op=ALU.add), r=["bdiag", "cmask"], w=["bdiag"])
    for bk in ("bdiag", "bprev"):
        c.op("dve", lambda e: e.tensor_tensor(out=P[bk][:], in0=P[bk][:], in1=P["cb31"][:].unsqueeze(2).to_broadcast([128, 16, 128]), op=ALU.subtract), r=[bk, "cb31"], w=[bk])
    lam4 = load("lam4", [128, 256], I["lam4"].partition_broadcast(128))
    P["subln"] = load("subln", [128, 128], I["subln"].partition_broadcast(128))
    c.op("dve", lambda e: e.tensor_scalar(out=P["subln"][:], in0=P["subln"][:], scalar1=1.0 - LAM_INIT, scalar2=None, op0=ALU.mult), r=["subln"], w=["subln"])
    P["convwT"] = load("convwT", [128, 12, 4], I["convwT"]); P["convb"] = load("convb", [128, 12], I["convb"])
    P["dtb"] = load("dtb", [128, 16], I["dtb"].partition_broadcast(128))
    alog = load("alog", [128, 16], I["alog"].partition_broadcast(128))
    P["dsk"] = load("dsk", [128, 16], I["dsk"].partition_broadcast(128))
    P["ssmnw"] = load("ssmnw", [128, 1024], I["ssmnw"].partition_broadcast(128))
    keysf = load("keysf", [128, 16, 128], I["keysT"])
    P["keysT"] = c.sb("keysT", [128, 16, 128], BF16)
    c.op("dve", lambda e: e.tensor_copy(P["keysT"][:], keysf[:]), r=["keysf"], w=["keysT"])
    P["aneg"] = c.sb("aneg", [128, 16], F32)
    c.op("act", lambda e: e.activation(out=P["aneg"][:], in_=alog[:], func=AF.Exp), r=["alog"], w=["aneg"])
    c.op("dve", lambda e: e.tensor_scalar(out=P["aneg"][:], in0=P["aneg"][:], scalar1=-1.0, scalar2=None, op0=ALU.mult), r=["aneg"], w=["aneg"])
    lp = c.sb("lp", [128, 128], F32); ls = c.sb("ls", [128, 2], F32)
    c.op("dve", lambda e: e.tensor_tensor(out=lp[:, 0:64], in0=lam4[:, 0:64], in1=lam4[:, 64:128], op=ALU.mult), r=["lam4"], w=["lp"])
    c.op("dve", lambda e: e.tensor_tensor(out=lp[:, 64:128], in0=lam4[:, 128:192], in1=lam4[:, 192:256], op=ALU.mult), r=["lam4", "lp"], w=["lp"])
    c.op("dve", lambda e: e.tensor_reduce(out=ls[:], in_=lp[:].rearrange("p (a b) -> p a b", a=2), axis=AX.X, op=ALU.add), r=["lp"], w=["ls"])
    c.op("act", lambda e: e.activation(out=ls[:], in_=ls[:], func=AF.Exp), r=["ls"], w=["ls"])
    P["neglam"] = c.sb("neglam", [128, 1], F32)
    c.op("dve", lambda e: e.tensor_tensor(out=P["neglam"][:], in0=ls[:, 1:2], in1=ls[:, 0:1], op=ALU.subtract), r=["ls"], w=["neglam"])
    c.op("dve", lambda e: e.tensor_scalar(out=P["neglam"][:], in0=P["neglam"][:], scalar1=-LAM_INIT, scalar2=None, op0=ALU.add), r=["neglam"], w=["neglam"])
    scf = c.sb("scf", [128, 16], F32)
    c.op("act", lambda e: e.activation(out=scf[:], in_=ccol[:], func=AF.Silu), r=["ccol"], w=["scf"])
    modT = c.sb("modT", [128, 96], F32)
    P["g1b"] = c.sb("g1b", [128, D], F32); P["g2b"] = c.sb("g2b", [128, D], F32)
    with contextlib.ExitStack() as pes:
        aw = [c.sb("aw%d" % i, [128, 16, 512], F32, pes) for i in range(2)]
        abr = [c.sb("abr%d" % i, [128, 512], F32, pes) for i in range(2)]
        psm = [c.ps("psm%d" % i, [128, 512], F32, pes) for i in range(2)]
        awv = I["ada_w"].rearrange("(c p) n -> p c n", p=128)
        for cb in range(24):
            bi = cb % 2
            c.dma("sp", aw[bi][:], awv[:, :, cb * 512:(cb + 1) * 512], w=["aw%d" % bi], stream="aw%d" % bi)
            grp = cb // 4
            if grp in (2, 5):
                c.dma("pool", abr[bi][:], I["ada_brow"][:, cb * 512:(cb + 1) * 512].partition_broadcast(128), w=["abr%d" % bi], stream="abr%d" % bi)
                for kc in range(16):
                    c.op("pe", lambda e: e.matmul(psm[bi][:], scf[:, kc:kc + 1].to_broadcast([128, 128]), aw[bi][:, kc, :], start=(kc == 0), stop=(kc == 15)),
                         r=["scf", "aw%d" % bi], w=["psm%d" % bi])
                dst = P["g1b"] if grp == 2 else P["g2b"]; dk = "g1b" if grp == 2 else "g2b"
                off = (cb % 4) * 512
                c.op("dve", lambda e: e.tensor_tensor(out=dst[:, off:off + 512], in0=psm[bi][:], in1=abr[bi][:], op=ALU.add), r=["psm%d" % bi, "abr%d" % bi, dk], w=[dk])
            else:
                for cc in range(4):
                    for kc in range(16):
                        c.op("pe", lambda e: e.matmul(psm[bi][:, cc:cc + 1], aw[bi][:, kc, cc * 128:(cc + 1) * 128], scf[:, kc:kc + 1], start=(kc == 0), stop=(kc == 15)),
                             r=["scf", "aw%d" % bi], w=["psm%d" % bi])
                c.op("dve", lambda e: e.tensor_tensor(out=modT[:, cb * 4:cb * 4 + 4], in0=psm[bi][:, 0:4], in1=adabT[:, cb * 4:cb * 4 + 4], op=ALU.add), r=["psm%d" % bi, "adabT", "modT"], w=["modT"])
        c.dma("sp", S["mod"], modT[:], r=["modT"], stream="modo")
        c.barrier()
    P["w1p"] = c.sb("w1p", [128, 16], F32); P["sh1T"] = c.sb("sh1T", [128, 16], F32)
    P["w2p"] = c.sb("w2p", [128, 16], F32); P["sh2T"] = c.sb("sh2T", [128, 16], F32)
    for (wp, sh, nT, nk, o) in ((P["w1p"], P["sh1T"], n1T, "n1T", 0), (P["w2p"], P["sh2T"], n2T, "n2T", 48)):
        c.op("dve", lambda e: e.tensor_scalar(out=wp[:], in0=modT[:, o + 16:o + 32], scalar1=1.0, scalar2=None, op0=ALU.add), r=["modT"], w=["wp%d" % o])
        c.op("dve", lambda e: e.tensor_tensor(out=wp[:], in0=wp[:], in1=nT[:], op=ALU.mult), r=["wp%d" % o, nk], w=["wp%d" % o])
        c.op("dve", lambda e: e.tensor_copy(sh[:], modT[:, o:o + 16]), r=["modT"], w=["sh%d" % o])
    c.barrier()
    c.sb = _sb0
    tes.close()


def cast_dram(c, src, dst, R, C, tag):
    with contextlib.ExitStack() as pes:
        CW = min(C, 2048)
        fin = [c.sb("cf%d" % i, [128, CW], F32, pes) for i in range(3)]
        fo = [c.sb("co%d" % i, [128, CW], BF16, pes) for i in range(3)]
        n = 0
        for r0 in range(0, R, 128):
            for c0 in range(0, C, CW):
                cw = min(CW, C - c0); bi = n % 3
                c.dma("sp", fin[bi][:, :cw], src[r0:r0 + 128, c0:c0 + cw], w=["cf%d" % bi], stream="cf%d" % bi)
                if n % 2 == 0:
                    c.op("dve", lambda e: e.tensor_copy(fo[bi][:, :cw], fin[bi][:, :cw]), r=["cf%d" % bi], w=["co%d" % bi])
                else:
                    c.op("act", lambda e: e.activation(out=fo[bi][:, :cw], in_=fin[bi][:, :cw], func=AF.Copy), r=["cf%d" % bi], w=["co%d" % bi])
                c.dma("pool", dst[r0:r0 + 128, c0:c0 + cw], fo[bi][:, :cw], r=["co%d" % bi], w=[tag], stream="co%d" % bi)
                n += 1
        c.barrier()


def phase_cast(c, cfg, I, S, P, stop_after):
    cast_dram(c, I["w_in"], S["w_in_bf"], D, 5648, "w_in_bf")


def cast_gen(c, fin, fo, jobs):
    n = 0
    for (src, dst, R, C, tag) in jobs:
        for r0 in range(0, R, 128):
            for c0 in range(0, C, 2048):
                cw = min(2048, C - c0); bi = n % 3; n += 1
                c.dma("sp", fin[bi][:, :cw], src[r0:r0 + 128, c0:c0 + cw], w=["gcf%d" % bi], stream="gcf%d" % bi)
                c.op("pool", lambda e: e.tensor_copy(fo[bi][:, :cw], fin[bi][:, :cw]), r=["gcf%d" % bi], w=["gco%d" % bi])
                c.dma("pool", dst[r0:r0 + 128, c0:c0 + cw], fo[bi][:, :cw], r=["gco%d" % bi], w=[tag], stream="gco%d" % bi)
                yield


def norm_transpose(c, pes_bufs, x_src_rows, wp, sh, wpk, shk, hnT, hnTk, tag):
    xt, junk, xs, ssq, pst, P = pes_bufs
    for i in range(4):
        bi = i % 2
        c.dma("sp", xt[bi][:], x_src_rows[i * 128:(i + 1) * 128, :], w=["xt%d" % bi], stream="xt%d" % bi)
        c.op("act", lambda e: e.activation(out=junk[:], in_=xt[bi][:], func=AF.Square, accum_out=ssq[:, i:i + 1]), r=["xt%d" % bi], w=["junk", "ssq"])
        c.op("dve", lambda e: e.tensor_scalar(out=ssq[:, i:i + 1], in0=ssq[:, i:i + 1], scalar1=1.0 / D, scalar2=EPS, op0=ALU.mult, op1=ALU.add), r=["ssq"], w=["ssq"])
        c.op("act", lambda e: e.activation(out=ssq[:, i:i + 1], in_=ssq[:, i:i + 1], func=AF.Sqrt), r=["ssq"], w=["ssq"])
        c.op("dve", lambda e: e.reciprocal(out=ssq[:, i:i + 1], in_=ssq[:, i:i + 1]), r=["ssq"], w=["ssq"])
        c.op("act", lambda e: e.activation(out=xs[:, i, :], in_=xt[bi][:], func=AF.Copy, scale=ssq[:, i:i + 1]), r=["xt%d" % bi, "ssq"], w=["xs"])
    for cp in range(8):
        pb = cp % 2
        for j in range(2):
            ch = cp * 2 + j
            for i in range(4):
                c.op("pe", lambda e: e.transpose(pst[pb][:, j * 512 + i * 128: j * 512 + (i + 1) * 128], xs[:, i, ch * 128:(ch + 1) * 128], P["ident"][:]),
                     r=["xs", "ident"], w=["pst%d" % pb])
        for j in range(2):
            ch = cp * 2 + j
            c.op("act", lambda e: e.activation(out=hnT[:, ch, :], in_=pst[pb][:, j * 512:(j + 1) * 512], func=AF.Identity, scale=wp[:, ch:ch + 1], bias=sh[:, ch:ch + 1]),
                 r=["pst%d" % pb, wpk, shk], w=[hnTk])


def phase1a(c, cfg, I, S, P):
    with contextlib.ExitStack() as pes:
        xt = [c.sb("xt%d" % i, [128, D], F32, pes) for i in range(2)]
        junk = c.sb("junk", [128, D], BF16, pes)
        xs = c.sb("xs", [128, 4, D], BF16, pes)
        ssq = c.sb("ssq", [128, 4], F32, pes)
        pst = [c.ps("pst%d" % i, [128, 1024], BF16, pes) for i in range(2)]
        hnTs = [c.sb("hnT%d" % i, [128, 16, 512], BF16, pes) for i in range(2)]
        wg = [c.sb("wg%d" % i, [128, 16, 512], BF16, pes) for i in range(2)]
        wdt = c.sb("wdt", [128, 16, 16], BF16, pes)
        psa = [c.ps("psa%d" % i, [128, 512], F32, pes) for i in range(3)]
        ps2 = c.ps("ps2", [128, 512], F32, pes)
        sq = c.sb("sq", [128, 512], BF16, pes); rs = c.sb("rs", [128, 512], F32, pes)
        ko = [c.sb("ko%d" % i, [128, 512], BF16, pes) for i in range(2)]
        vo = c.sb("vo", [128, 8, 4, 129], BF16, pes)
        zo = c.sb("zo", [128, 4, 1024], BF16, pes)
        uo = [c.sb("uo%d" % i, [128, 512], F32, pes) for i in range(2)]
        dto = c.sb("dto", [128, 4, 16], F32, pes)
        wv = S["w_in_bf"].rearrange("(c p) n -> p c n", p=128)
        c.dma("sp", wdt[:], wv[:, :, 5632:5648], r=["w_in_bf"], w=["wdt"], stream="wdt")
        bufs = (xt, junk, xs, ssq, pst, P)
        state = {"wn": 0, "pn": 0, "kn": 0, "un": 0}
        def load_w(col0):
            bi = state["wn"] % 2; state["wn"] += 1
            c.dma("sp", wg[bi][:], wv[:, :, col0:col0 + 512], r=["w_in_bf"], w=["wg%d" % bi], stream="wg%d" % bi)
            return wg[bi], "wg%d" % bi
        def next_ps():
            bi = state["pn"] % 3; state["pn"] += 1
            return psa[bi], "psa%d" % bi
        def NT(b_):
            norm_transpose(c, bufs, I["xloc"][b_ * 512:(b_ + 1) * 512, :], P["w1p"], P["sh1T"], "wp0", "sh0", hnTs[b_ % 2], "hnT%d" % (b_ % 2), "a")
        NT(0)
        for blk in range(cfg.NBLK):
            own = blk >= cfg.NBLK - cfg.OWNBLK
            oblk = blk - (cfg.NBLK - cfg.OWNBLK)
            vcol = P["validcol"][:, blk:blk + 1]
            if blk + 1 < cfg.NBLK: NT(blk + 1)
            hnT = hnTs[blk % 2]; HK = "hnT%d" % (blk % 2)
            fm = [("k", 1024), ("k", 1536)] + ([("q", 0), ("q", 512)] if own else [])
            for (kind, col0) in fm:
                w, wk = load_w(col0)
                for cc in range(4):
                    hh = (col0 % 1024) // 128 + cc
                    ps, pk = next_ps()
                    for kc in range(16):
                        c.op("pe", lambda e: e.matmul(ps[:], w[:, kc, cc * 128:(cc + 1) * 128], hnT[:, kc, :], start=(kc == 0), stop=(kc == 15)), r=[wk, HK], w=[pk])
                    c.op("act", lambda e: e.activation(out=sq[:], in_=ps[:], func=AF.Square), r=[pk], w=["sq"])
                    c.op("pe", lambda e: e.matmul(ps2[:], P["blockones"][:], sq[:], start=True, stop=True), r=["sq", "blockones"], w=["ps2"])
                    c.op("dve", lambda e: e.tensor_scalar(out=rs[:], in0=ps2[:], scalar1=64.0 * EPS, scalar2=None, op0=ALU.add), r=["ps2"], w=["rs"])
                    c.op("act", lambda e: e.activation(out=rs[:], in_=rs[:], func=AF.Sqrt), r=["rs"], w=["rs"])
                    c.op("dve", lambda e: e.reciprocal(out=rs[:], in_=rs[:]), r=["rs"], w=["rs"])
                    ki = state["kn"] % 2; state["kn"] += 1
                    wn = P["knw8"] if kind == "k" else P["qnw"]
                    c.op("dve", lambda e: e.scalar_tensor_tensor(out=ko[ki][:], in0=ps[:], scalar=wn[:, 0:1], in1=rs[:], op0=ALU.mult, op1=ALU.mult),
                         r=[pk, "rs", "knw8", "qnw"], w=["ko%d" % ki])
                    if kind == "k":
                        c.dma("pool", S["kT"][hh, :, blk * 512:(blk + 1) * 512], ko[ki][:], r=["ko%d" % ki], w=["kT_d"], stream="ko%d" % ki)
                    else:
                        c.dma("pool", S["qT"][hh, :, oblk * 512:(oblk + 1) * 512], ko[ki][:], r=["ko%d" % ki], w=["qT_d"], stream="ko%d" % ki)
            for g in range(3):
                w, wk = load_w(4096 + g * 512)
                for cc in range(4):
                    ch = g * 4 + cc
                    ps, pk = next_ps()
                    for kc in range(16):
                        c.op("pe", lambda e: e.matmul(ps[:], w[:, kc, cc * 128:(cc + 1) * 128], hnT[:, kc, :], start=(kc == 0), stop=(kc == 15)), r=[wk, HK], w=[pk])
                    ui = state["un"] % 2; state["un"] += 1
                    c.op("dve", lambda e: e.tensor_scalar(out=uo[ui][:], in0=ps[:], scalar1=vcol, scalar2=None, op0=ALU.mult), r=[pk, "validcol"], w=["uo%d" % ui])
                    c.dma("pool", S["uT"][ch, :, blk * 512:(blk + 1) * 512], uo[ui][:], r=["uo%d" % ui], w=["uT_d"], stream="uo%d" % ui)
            c.op("dve", lambda e: e.tensor_copy(vo[:, :, :, 128:129].rearrange("p a b c -> p (a b c)"), vcol.to_broadcast([128, 32])), r=["validcol"], w=["vo"])
            for g in range(2):
                w, wk = load_w(2048 + g * 512)
                for i in range(4):
                    ps, pk = next_ps()
                    for kc in range(16):
                        c.op("pe", lambda e: e.matmul(ps[:], hnT[:, kc, i * 128:(i + 1) * 128], w[:, kc, :], start=(kc == 0), stop=(kc == 15)), r=[wk, HK], w=[pk])
                    c.op("dve", lambda e: e.tensor_scalar(out=vo[:, g * 4:(g + 1) * 4, i, 0:128], in0=ps[:].rearrange("p (a b) -> p a b", a=4), scalar1=vcol, scalar2=None, op0=ALU.mult),
                         r=[pk, "validcol", "vo"], w=["vo"])
            for hh in range(8):
                c.dma("pool", S["v"][hh, :, blk * 4:(blk + 1) * 4, :], vo[:, hh, :, :], r=["vo"], w=["v_d"], stream="vo")
            ps, pk = next_ps()
            for i in range(4):
                for kc in range(16):
                    c.op("pe", lambda e: e.matmul(ps[:, i * 16:(i + 1) * 16], hnT[:, kc, i * 128:(i + 1) * 128], wdt[:, kc, :], start=(kc == 0), stop=(kc == 15)), r=["wdt", HK], w=[pk])
            c.op("dve", lambda e: e.tensor_copy(dto[:].rearrange("p a b -> p (a b)"), ps[:, 0:64]), r=[pk], w=["dto"])
            c.dma("pool", S["dt"][blk * 512:(blk + 1) * 512, :].rearrange("(n p) c -> p n c", p=128), dto[:], r=["dto"], w=["dt_d"], stream="dto")
            if own:
                for g in range(2):
                    w, wk = load_w(3072 + g * 512)
                    for i in range(4):
                        ps, pk = next_ps()
                        for kc in range(16):
                            c.op("pe", lambda e: e.matmul(ps[:], hnT[:, kc, i * 128:(i + 1) * 128], w[:, kc, :], start=(kc == 0), stop=(kc == 15)), r=[wk, HK], w=[pk])
                        c.op("act", lambda e: e.activation(out=zo[:, i, g * 512:(g + 1) * 512], in_=ps[:], func=AF.Silu), r=[pk, "zo"], w=["zo"])
                c.dma("pool", S["z"][oblk * 512:(oblk + 1) * 512, :].rearrange("(n p) c -> p n c", p=128), zo[:], r=["zo"], w=["z_d"], stream="zo")
        c.barrier()


def phase1b(c, cfg, I, S, P):
    with contextlib.ExitStack() as pes:
        U = c.sb("U", [128, 12, 515], F32, pes)
        acc = [c.sb("acc%d" % i, [128, 512], F32, pes) for i in range(2)]
        xbcT = c.sb("xbcT", [128, 12, 512], BF16, pes)
        state_f = c.sb("state_f", [128, 2, 512], F32, pes); state_b = c.sb("state_b", [128, 2, 512], BF16, pes)
        dtr = c.sb("dtr", [128, 4, 16], F32, pes); dtv = c.sb("dtv", [128, 4, 16], F32, pes); dta = c.sb("dta", [128, 4, 16], F32, pes)
        acs = c.sb("acs", [128, 32], F32, pes); ein = c.sb("ein", [128, 48], F32, pes); dec = c.sb("dec", [128, 48], F32, pes)
        xtok = c.sb("xtok", [128, 1024], BF16, pes); xc = c.sb("xc", [128, 1024], BF16, pes); xcd = c.sb("xcd", [128, 1024], BF16, pes)
        Btok = c.sb("Btok", [128, 2, 128], BF16, pes)
        cbm = c.sb("cbm", [128, 2, 128], F32, pes)
        diag = c.sb("diag", [128, 16, 128], F32, pes); seg = c.sb("seg", [128, 16, 128], F32, pes)
        Mt = c.sb("Mt", [128, 16, 128], BF16, pes)
        t1 = c.sb("t1", [128, 1024], F32, pes); t2 = c.sb("t2", [128, 1024], F32, pes)
        zs = c.sb("zs", [128, 1024], BF16, pes); yo = c.sb("yo", [128, 1024], BF16, pes)
        junk2 = c.sb("junk2", [128, 512], BF16, pes); ssg = c.sb("ssg", [128, 2], F32, pes)
        b0 = c.ps("b0", [128, 512], F32, pes)
        b1 = c.ps("b1", [128, 1024], BF16, pes); b2 = c.ps("b2", [128, 1024], BF16, pes)
        b34 = [c.ps("b3", [128, 512], F32, pes), c.ps("b4", [128, 512], F32, pes)]
        b56 = [c.ps("b5", [128, 512], F32, pes), c.ps("b6", [128, 512], F32, pes)]
        b7 = c.ps("b7", [128, 512], F32, pes)
        c.op("dve", lambda e: e.memset(U[:, :, 0:3], 0.0), w=["U"])
        c.op("dve", lambda e: e.memset(state_f[:], 0.0), w=["state_f"])
        c.op("dve", lambda e: e.memset(state_b[:], 0.0), w=["state_b"])
        for blk in range(cfg.NBLK):
            own = blk >= cfg.NBLK - cfg.OWNBLK
            oblk = blk - (cfg.NBLK - cfg.OWNBLK)
            vcol = P["validcol"][:, blk:blk + 1]
            c.dma("sp", U[:, :, 3:515], S["uT"][:, :, blk * 512:(blk + 1) * 512].rearrange("c p t -> p c t"), r=["uT_d"], w=["U"], stream="U")
            c.dma("sp", dtr[:], S["dt"][blk * 512:(blk + 1) * 512, :].rearrange("(n p) c -> p n c", p=128), r=["dt_d"], w=["dtr"], stream="dtr")
            for ch in range(12):
                a = acc[ch % 2]; ak = "acc%d" % (ch % 2)
                c.op("dve", lambda e: e.tensor_scalar(out=a[:], in0=U[:, ch, 0:512], scalar1=P["convwT"][:, ch, 0:1], scalar2=P["convb"][:, ch:ch + 1], op0=ALU.mult, op1=ALU.add),
                     r=["U", "convwT", "convb"], w=[ak])
                for k in range(1, 4):
                    c.op("dve", lambda e: e.scalar_tensor_tensor(out=a[:], in0=U[:, ch, k:k + 512], scalar=P["convwT"][:, ch, k:k + 1], in1=a[:], op0=ALU.mult, op1=ALU.add),
                         r=["U", "convwT", ak], w=[ak])
                c.op("act", lambda e: e.activation(out=xbcT[:, ch, :], in_=a[:], func=AF.Silu), r=[ak], w=["xbcT"])
            c.op("pool", lambda e: e.tensor_copy(U[:, :, 0:3], U[:, :, 512:515]), r=["U"], w=["U"])
            c.op("dve", lambda e: e.tensor_tensor(out=dtv[:], in0=dtr[:], in1=P["dtb"][:].unsqueeze(1).to_broadcast([128, 4, 16]), op=ALU.add), r=["dtr", "dtb"], w=["dtv"])
            c.op("act", lambda e: e.activation(out=dtv[:], in_=dtv[:], func=AF.Exp), r=["dtv"], w=["dtv"])
            c.op("act", lambda e: e.activation(out=dtv[:], in_=dtv[:], func=AF.Ln, bias=1.0), r=["dtv"], w=["dtv"])
            c.op("dve", lambda e: e.tensor_scalar(out=dtv[:], in0=dtv[:], scalar1=vcol, scalar2=None, op0=ALU.mult), r=["dtv", "validcol"], w=["dtv"])
            c.op("dve", lambda e: e.tensor_tensor(out=dta[:], in0=dtv[:], in1=P["aneg"][:].unsqueeze(1).to_broadcast([128, 4, 16]), op=ALU.mult), r=["dtv", "aneg"], w=["dta"])
            for i in range(4):
                ts = slice(i * 128, (i + 1) * 128)
                c.op("pe", lambda e: e.matmul(b0[:, 0:16], P["tri"][:], dta[:, i, :], start=True, stop=True), r=["tri", "dta"], w=["b0"])
                c.op("pe", lambda e: e.matmul(b0[:, 16:32], P["ones"][:], dta[:, i, :], start=True, stop=True), r=["ones", "dta", "b0"], w=["b0"])
                c.op("dve", lambda e: e.tensor_copy(acs[:], b0[:, 0:32]), r=["b0"], w=["acs"])
                c.op("dve", lambda e: e.tensor_tensor(out=ein[:, 0:16], in0=acs[:, 16:32], in1=acs[:, 0:16], op=ALU.subtract), r=["acs"], w=["ein"])
                c.op("dve", lambda e: e.tensor_copy(ein[:, 16:32], acs[:, 16:32]), r=["acs", "ein"], w=["ein"])
                c.op("dve", lambda e: e.tensor_copy(ein[:, 32:48], acs[:, 0:16]), r=["acs", "ein"], w=["ein"])
                c.op("act", lambda e: e.activation(out=dec[:], in_=ein[:], func=AF.Exp), r=["ein"], w=["dec"])
                for ch in range(8):
                    c.op("pe", lambda e: e.transpose(b1[:, ch * 128:(ch + 1) * 128], xbcT[:, ch, ts], P["ident"][:]), r=["xbcT", "ident"], w=["b1"])
                for g in range(2):
                    c.op("pe", lambda e: e.transpose(b2[:, g * 128:(g + 1) * 128], xbcT[:, 8 + g, ts], P["ident"][:]), r=["xbcT", "ident"], w=["b2"])
                c.op("act", lambda e: e.activation(out=xtok[:], in_=b1[:], func=AF.Copy), r=["b1"], w=["xtok"])
                c.op("dve", lambda e: e.tensor_copy(Btok[:].rearrange("p a b -> p (a b)"), b2[:, 0:256]), r=["b2"], w=["Btok"])
                c.op("dve", lambda e: e.tensor_tensor(out=xc[:].rearrange("p (a b) -> p a b", a=16), in0=xtok[:].rearrange("p (a b) -> p a b", a=16),
                                                       in1=dtv[:, i, :].unsqueeze(2).to_broadcast([128, 16, 64]), op=ALU.mult), r=["xtok", "dtv"], w=["xc"])
                c.op("pool", lambda e: e.tensor_tensor(out=xcd[:].rearrange("p (a b) -> p a b", a=16), in0=xc[:].rearrange("p (a b) -> p a b", a=16),
                                                        in1=dec[:, 0:16].unsqueeze(2).to_broadcast([128, 16, 64]), op=ALU.mult), r=["xc", "dec"], w=["xcd"])
                if own:
                    row0 = oblk * 512 + i * 128
                    c.dma("sp", zs[:], S["z"][row0:row0 + 128, :], r=["z_d"], w=["zs"], stream="zs")
                    for g in range(2):
                        c.op("pe", lambda e: e.matmul(b0[:, 128 + g * 128:256 + g * 128], xbcT[:, 8 + g, ts], xbcT[:, 10 + g, ts], start=True, stop=True), r=["xbcT", "b0"], w=["b0"])
                    c.op("dve", lambda e: e.tensor_tensor(out=cbm[:], in0=b0[:, 128:384].rearrange("p (a b) -> p a b", a=2), in1=P["tri"][:].unsqueeze(1).to_broadcast([128, 2, 128]), op=ALU.mult),
                         r=["b0", "tri"], w=["cbm"])
                    c.op("pool", lambda e: e.tensor_tensor(out=diag[:], in0=P["identf"][:].unsqueeze(1).to_broadcast([128, 16, 128]), in1=acs[:, 0:16].unsqueeze(2).to_broadcast([128, 16, 128]), op=ALU.mult),
                         r=["identf", "acs"], w=["diag"])
                    for k4 in range(4):
                        pb = b34[k4 % 2]; pk = "b%d" % (3 + k4 % 2)
                        c.op("pe", lambda e: e.matmul(pb[:], P["ones"][:], diag[:, k4 * 4:(k4 + 1) * 4, :].rearrange("p a b -> p (a b)"), start=True, stop=True), r=["ones", "diag"], w=[pk])
                        for jj in range(4):
                            j = k4 * 4 + jj
                            c.op("dve", lambda e: e.tensor_scalar(out=seg[:, j, :], in0=pb[:, jj * 128:(jj + 1) * 128], scalar1=acs[:, j:j + 1], scalar2=0.0, op0=ALU.subtract, op1=ALU.min),
                                 r=[pk, "acs", "seg"], w=["seg"])
                    c.op("act", lambda e: e.activation(out=seg[:].rearrange("p a b -> p (a b)"), in_=seg[:].rearrange("p a b -> p (a b)"), func=AF.Exp), r=["seg"], w=["seg"])
                    for g in range(2):
                        c.op("dve", lambda e: e.tensor_tensor(out=Mt[:, 8 * g:8 * g + 8, :], in0=seg[:, 8 * g:8 * g + 8, :], in1=cbm[:, g, :].unsqueeze(1).to_broadcast([128, 8, 128]), op=ALU.mult),
                             r=["seg", "cbm", "Mt"], w=["Mt"])
                    for hh in range(16):
                        pb = b56[hh // 8]; pk = "b%d" % (5 + hh // 8)
                        c.op("pe", lambda e: e.matmul(pb[:, (hh % 8) * 64:(hh % 8 + 1) * 64], Mt[:, hh, :], xc[:, hh * 64:(hh + 1) * 64], start=True, stop=True), r=["Mt", "xc", pk], w=[pk])
                    for g in range(2):
                        gs = slice(g * 512, (g + 1) * 512)
                        c.op("pe", lambda e: e.matmul(b7[:], xbcT[:, 10 + g, ts], state_b[:, g, :], start=True, stop=True), r=["xbcT", "state_b"], w=["b7"])
                        c.op("dve", lambda e: e.tensor_tensor(out=t1[:, gs].rearrange("p (a b) -> p a b", a=8), in0=b7[:].rearrange("p (a b) -> p a b", a=8),
                                                               in1=dec[:, 32 + 8 * g:40 + 8 * g].unsqueeze(2).to_broadcast([128, 8, 64]), op=ALU.mult), r=["b7", "dec", "t1"], w=["t1"])
                        c.op("dve", lambda e: e.tensor_tensor(out=t1[:, gs], in0=t1[:, gs], in1=b56[g][:], op=ALU.add), r=["t1", "b%d" % (5 + g)], w=["t1"])
                    c.op("pool", lambda e: e.tensor_tensor(out=t2[:].rearrange("p (a b) -> p a b", a=16), in0=xtok[:].rearrange("p (a b) -> p a b", a=16),
                                                            in1=P["dsk"][:].unsqueeze(2).to_broadcast([128, 16, 64]), op=ALU.mult), r=["xtok", "dsk"], w=["t2"])
                    c.op("dve", lambda e: e.tensor_tensor(out=t1[:], in0=t1[:], in1=t2[:], op=ALU.add), r=["t1", "t2"], w=["t1"])
                    c.op("dve", lambda e: e.tensor_tensor(out=t1[:], in0=t1[:], in1=zs[:], op=ALU.mult), r=["t1", "zs"], w=["t1"])
                    for g in range(2):
                        c.op("act", lambda e: e.activation(out=junk2[:], in_=t1[:, g * 512:(g + 1) * 512], func=AF.Square, accum_out=ssg[:, g:g + 1]), r=["t1", "ssg"], w=["junk2", "ssg"])
                    c.op("dve", lambda e: e.tensor_scalar(out=ssg[:], in0=ssg[:], scalar1=1.0 / 512, scalar2=EPS, op0=ALU.mult, op1=ALU.add), r=["ssg"], w=["ssg"])
                    c.op("act", lambda e: e.activation(out=ssg[:], in_=ssg[:], func=AF.Sqrt), r=["ssg"], w=["ssg"])
                    c.op("dve", lambda e: e.reciprocal(out=ssg[:], in_=ssg[:]), r=["ssg"], w=["ssg"])
                    for g in range(2):
                        gs = slice(g * 512, (g + 1) * 512)
                        c.op("dve", lambda e: e.scalar_tensor_tensor(out=yo[:, gs], in0=t1[:, gs], scalar=ssg[:, g:g + 1], in1=P["ssmnw"][:, gs], op0=ALU.mult, op1=ALU.mult),
                             r=["t1", "ssg", "ssmnw", "yo"], w=["yo"])
                    c.dma("pool", S["mix"][row0:row0 + 128, 1024:2048], yo[:], r=["yo"], w=["mix_d"], stream="yo")
                for g in range(2):
                    pb = b34[g]; pk = "b%d" % (3 + g)
                    c.op("pe", lambda e: e.matmul(pb[:], Btok[:, g, :], xcd[:, g * 512:(g + 1) * 512], start=True, stop=True), r=["Btok", "xcd"], w=[pk])
                    c.op("dve", lambda e: e.tensor_tensor(out=state_f[:, g, :].rearrange("p (a b) -> p a b", a=8), in0=state_f[:, g, :].rearrange("p (a b) -> p a b", a=8),
                                                           in1=dec[:, 16 + 8 * g:24 + 8 * g].unsqueeze(2).to_broadcast([128, 8, 64]), op=ALU.mult), r=["state_f", "dec"], w=["state_f"])
                    c.op("dve", lambda e: e.tensor_tensor(out=state_f[:, g, :], in0=state_f[:, g, :], in1=pb[:], op=ALU.add), r=["state_f", pk], w=["state_f"])
                c.op("act", lambda e: e.activation(out=state_b[:].rearrange("p a b -> p (a b)"), in_=state_f[:].rearrange("p a b -> p (a b)"), func=AF.Copy), r=["state_f"], w=["state_b"])
        c.barrier()


def phase_attn(c, cfg, I, S, P):
    T0 = cfg.T0; QB = cfg.OWNBLK
    with contextlib.ExitStack() as pes:
        kT = c.sb("kT", [128, cfg.SLOC], BF16, pes); v = c.sb("v", [128, cfg.NT, 129], BF16, pes); qT = c.sb("qT", [128, cfg.QTR], BF16, pes)
        pT = [c.sb("pT%d" % i, [128, 2, 512], BF16, pes) for i in range(2)]
        tmpb = [c.sb("tmpb%d" % i, [128, 128], F32, pes) for i in range(2)]
        pn = [c.sb("pn%d" % i, [128, 128], BF16, pes) for i in range(2)]
        pslb = [c.ps("pslb%d" % i, [128, 1024], F32, pes) for i in range(2)]
        psn = c.ps("psn", [128, 512], F32, pes)
        pacc = [c.ps("pacc%d" % i, [128, 512], F32, pes) for i in range(3)]
        r12 = c.sb("r12", [128, 2], F32, pes); nl = c.sb("nl", [128, 1], F32, pes)
        o1 = c.sb("o1", [128, 128], F32, pes); dd = c.sb("dd", [128, 128], F32, pes); junk3 = c.sb("junk3", [128, 128], BF16, pes)
        ssa = c.sb("ssa", [128, 1], F32, pes)
        atto = c.sb("atto", [128, 4, 128], BF16, pes)
        def acc(m, a):
            idx = m * 4 + a
            return pacc[idx // 3][:, (idx % 3) * 129:(idx % 3) * 129 + 129], "pacc%d" % (idx // 3)
        nn = 0
        gfin = [c.sb("gcf%d" % i, [128, 2048], F32, pes) for i in range(3)]
        gfo = [c.sb("gco%d" % i, [128, 2048], BF16, pes) for i in range(3)]
        cgen = cast_gen(c, gfin, gfo, [(I["w_out"], S["w_out_bf"], D, D, "w_out_bf"), (I["peer_wq"], S["wq_bf"], D, D, "wq_bf"),
                                       (I["downT"], S["downT_bf"], D, NEXP, "downT_bf"), (I["up"], S["up_bf"], NEXP, D, "up_bf")])
        per_step = -(-(16 + 16 + 128 + 128) // (H * QB))
        for h in range(H):
            c.dma("sp", kT[:], S["kT"][h], r=["kT_d"], w=["kT"], stream="kT")
            c.dma("sp", v[:], S["v"][h], r=["v_d"], w=["v"], stream="v")
            c.dma("sp", qT[:], S["qT"][h], r=["qT_d"], w=["qT"], stream="qT")
            for qb in range(QB):
                nfar = T0 + 4 * qb - 1
                started = set()
                def L_(kt):
                    pb = kt % 2
                    for m in range(2):
                        ms = slice(m * 64, (m + 1) * 64)
                        c.op("pe", lambda e: e.matmul(pslb[pb][:, m * 512:(m + 1) * 512], kT[ms, kt * 128:(kt + 1) * 128], qT[ms, qb * 512:(qb + 1) * 512], start=True, stop=True),
                             r=["kT", "qT", "pslb%d" % pb], w=["pslb%d" % pb])
                def X_(kt):
                    pb = kt % 2
                    c.op("act", lambda e: e.activation(out=pT[pb][:].rearrange("p a b -> p (a b)"), in_=pslb[pb][:], func=AF.Exp),
                         r=["pslb%d" % pb], w=["pT%d_0" % pb, "pT%d_1" % pb])
                def PV_(kt):
                    pb = kt % 2
                    for m in range(2):
                        for a in range(4):
                            ap_, ak = acc(m, a)
                            c.op("pe", lambda e: e.matmul(ap_, pT[pb][:, m, a * 128:(a + 1) * 128], v[:, kt, :], start=((m, a) not in started), stop=False),
                                 r=["pT%d_%d" % (pb, m), "v", ak], w=[ak])
                            started.add((m, a))
                L_(0)
                for kt in range(nfar):
                    if kt + 1 < nfar: L_(kt + 1)
                    X_(kt)
                    PV_(kt)
                for a in range(4):
                    qt = T0 + 4 * qb + a
                    for kt in range(T0 + 4 * qb - 1, qt + 1):
                        diff = qt - kt
                        for m in range(2):
                            ms = slice(m * 64, (m + 1) * 64)
                            sl = nn % 4; bi = nn % 2; nn += 1
                            pv = psn[:, sl * 128:(sl + 1) * 128]
                            c.op("pe", lambda e: e.matmul(pv, kT[ms, kt * 128:(kt + 1) * 128], qT[ms, qb * 512 + a * 128:qb * 512 + (a + 1) * 128], start=True, stop=True),
                                 r=["kT", "qT"], w=["psn%d" % sl])
                            if diff <= 1:
                                bt = P["bdiag"] if diff == 0 else P["bprev"]
                                c.op("dve", lambda e: e.tensor_tensor(out=tmpb[bi][:], in0=pv, in1=bt[:, h * 2 + m, :], op=ALU.add), r=["psn%d" % sl, "bdiag", "bprev"], w=["tmpb%d" % bi])
                                c.op("act", lambda e: e.activation(out=pn[bi][:], in_=tmpb[bi][:], func=AF.Exp), r=["tmpb%d" % bi], w=["pn%d" % bi])
                            else:
                                c.op("act", lambda e: e.activation(out=pn[bi][:], in_=pv, func=AF.Exp), r=["psn%d" % sl], w=["pn%d" % bi])
                            ap_, ak = acc(m, a)
                            c.op("pe", lambda e: e.matmul(ap_, pn[bi][:], v[:, kt, :], start=((m, a) not in started), stop=(diff == 0)), r=["pn%d" % bi, "v", ak], w=[ak])
                            started.add((m, a))
                for a in range(4):
                    a1, k1 = acc(0, a); a2, k2 = acc(1, a)
                    c.op("dve", lambda e: e.reciprocal(out=r12[:, 0:1], in_=a1[:, 128:129]), r=[k1, "r12"], w=["r12"])
                    c.op("dve", lambda e: e.reciprocal(out=r12[:, 1:2], in_=a2[:, 128:129]), r=[k2, "r12"], w=["r12"])
                    c.op("dve", lambda e: e.tensor_tensor(out=nl[:], in0=r12[:, 1:2], in1=P["neglam"][:], op=ALU.mult), r=["r12", "neglam"], w=["nl"])
                    c.op("dve", lambda e: e.tensor_scalar(out=o1[:], in0=a1[:, 0:128], scalar1=r12[:, 0:1], scalar2=None, op0=ALU.mult), r=[k1, "r12"], w=["o1"])
                    c.op("dve", lambda e: e.scalar_tensor_tensor(out=dd[:], in0=a2[:, 0:128], scalar=nl[:, 0:1], in1=o1[:], op0=ALU.mult, op1=ALU.add), r=[k2, "nl", "o1"], w=["dd"])
                    c.op("act", lambda e: e.activation(out=junk3[:], in_=dd[:], func=AF.Square, accum_out=ssa[:, 0:1]), r=["dd"], w=["junk3", "ssa"])
                    c.op("dve", lambda e: e.tensor_scalar(out=ssa[:], in0=ssa[:], scalar1=1.0 / 128, scalar2=EPS, op0=ALU.mult, op1=ALU.add), r=["ssa"], w=["ssa"])
                    c.op("act", lambda e: e.activation(out=ssa[:], in_=ssa[:], func=AF.Sqrt), r=["ssa"], w=["ssa"])
                    c.op("dve", lambda e: e.reciprocal(out=ssa[:], in_=ssa[:]), r=["ssa"], w=["ssa"])
                    c.op("dve", lambda e: e.scalar_tensor_tensor(out=atto[:, a, :], in0=dd[:], scalar=ssa[:, 0:1], in1=P["subln"][:], op0=ALU.mult, op1=ALU.mult), r=["dd", "ssa", "subln", "atto"], w=["atto"])
                c.dma("sp", S["mix"][qb * 512:(qb + 1) * 512, h * 128:(h + 1) * 128].rearrange("(n p) c -> p n c", p=128), atto[:], r=["atto"], w=["mix_d"], stream="atto")
                for _ in range(per_step):
                    next(cgen, None)
        for _ in cgen:
            pass
        c.barrier()


def phase_out(c, cfg, I, S, P, y_out):
    xown0 = cfg.SLOC - cfg.QTR
    with contextlib.ExitStack() as pes:
        wo = c.sb("wo", [128, 16, D], BF16, pes)
        c.dma("sp", wo[:], S["w_out_bf"].rearrange("(c p) n -> p c n", p=128), r=["w_out_bf"], w=["wo"], stream="wo")
        mx = [c.sb("mx%d" % i, [128, D], BF16, pes) for i in range(2)]
        mixT = c.sb("mixT", [128, 16, 512], BF16, pes)
        xt = [c.sb("xta%d" % i, [128, D], F32, pes) for i in range(2)]
        ht = [c.sb("ht%d" % i, [128, D], F32, pes) for i in range(2)]
        ptr = [c.ps("ptr%d" % i, [128, 1024], BF16, pes) for i in range(2)]
        pso = [c.ps("pso%d" % i, [128, 512], F32, pes) for i in range(4)]
        for ob in range(cfg.OWNBLK):
            for i in range(4):
                bi = i % 2
                r0 = ob * 512 + i * 128
                c.dma("sp", mx[bi][:], S["mix"][r0:r0 + 128, :], r=["mix_d"], w=["mx%d" % bi], stream="mx%d" % bi)
                for half in range(2):
                    for j in range(8):
                        ch = half * 8 + j
                        c.op("pe", lambda e: e.transpose(ptr[half][:, j * 128:(j + 1) * 128], mx[bi][:, ch * 128:(ch + 1) * 128], P["ident"][:]), r=["mx%d" % bi, "ident"], w=["ptr%d" % half])
                    eng = "act" if half == 0 else "dve"
                    if eng == "act":
                        c.op("act", lambda e: e.activation(out=mixT[:, half * 8:half * 8 + 8, i * 128:(i + 1) * 128], in_=ptr[half][:].rearrange("p (a b) -> p a b", a=8), func=AF.Copy), r=["ptr%d" % half, "mixT"], w=["mixT"])
                    else:
                        c.op("dve", lambda e: e.tensor_copy(mixT[:, half * 8:half * 8 + 8, i * 128:(i + 1) * 128], ptr[half][:].rearrange("p (a b) -> p a b", a=8)), r=["ptr%d" % half, "mixT"], w=["mixT"])
            for i in range(4):
                bi = i % 2
                r0 = ob * 512 + i * 128
                c.dma("sp", xt[bi][:], I["xloc"][xown0 + r0:xown0 + r0 + 128, :], w=["xta%d" % bi], stream="xta%d" % bi)
                for db in range(4):
                    for kc in range(16):
                        c.op("pe", lambda e: e.matmul(pso[db][:], mixT[:, kc, i * 128:(i + 1) * 128], wo[:, kc, db * 512:(db + 1) * 512], start=(kc == 0), stop=(kc == 15)), r=["mixT", "wo"], w=["pso%d" % db])
                    ds_ = slice(db * 512, (db + 1) * 512)
                    c.op("dve", lambda e: e.tensor_tensor(out=ht[bi][:, ds_], in0=pso[db][:], in1=P["g1b"][:, ds_], op=ALU.mult), r=["pso%d" % db, "g1b", "ht%d" % bi], w=["ht%d" % bi])
                    c.op("pool", lambda e: e.tensor_tensor(out=ht[bi][:, ds_], in0=ht[bi][:, ds_], in1=xt[bi][:, ds_], op=ALU.add), r=["ht%d" % bi, "xta%d" % bi], w=["ht%d" % bi])
                c.dma("pool", S["h"][r0:r0 + 128, :], ht[bi][:], r=["ht%d" % bi], w=["h_d"], stream="ht%d" % bi)
        c.barrier()
    with contextlib.ExitStack() as pes:
        hn2T = c.sb("hn2T", [128, 16, 512], BF16, pes)
        for ob in range(cfg.OWNBLK):
            with contextlib.ExitStack() as aes:
                qpT = c.sb("qpT", [128, 16, 512], BF16, aes)
                with contextlib.ExitStack() as a1:
                    xt = [c.sb("xtb%d" % i, [128, D], F32, a1) for i in range(2)]
                    junk = c.sb("junkb", [128, D], BF16, a1); xs = c.sb("xsb", [128, 4, D], BF16, a1); ssq = c.sb("ssqb", [128, 4], F32, a1)
                    pst = [c.ps("pstb%d" % i, [128, 1024], BF16, a1) for i in range(2)]
                    norm_transpose(c, (xt, junk, xs, ssq, pst, P), S["h"][ob * 512:(ob + 1) * 512, :], P["w2p"], P["sh2T"], "wp48", "sh48", hn2T, "hn2T", "b")
                    c.barrier()
                with contextlib.ExitStack() as a2:
                    wg = [c.sb("wqg%d" % i, [128, 16, 512], BF16, a2) for i in range(2)]
                    psq = [c.ps("psq%d" % i, [128, 512], F32, a2) for i in range(2)]
                    wv = S["wq_bf"].rearrange("(c p) n -> p c n", p=128)
                    for g in range(4):
                        c.dma("sp", wg[g % 2][:], wv[:, :, g * 512:(g + 1) * 512], r=["wq_bf"], w=["wqg%d" % (g % 2)], stream="wqg%d" % (g % 2))
                        for cc in range(4):
                            ch = g * 4 + cc; pb = ch % 2
                            for kc in range(16):
                                c.op("pe", lambda e: e.matmul(psq[pb][:], wg[g % 2][:, kc, cc * 128:(cc + 1) * 128], hn2T[:, kc, :], start=(kc == 0), stop=(kc == 15)), r=["wqg%d" % (g % 2), "hn2T"], w=["psq%d" % pb])
                            c.op("act", lambda e: e.activation(out=qpT[:, ch, :], in_=psq[pb][:], func=AF.Copy), r=["psq%d" % pb, "qpT"], w=["qpT"])
                    c.barrier()
                with contextlib.ExitStack() as a3:
                    Ssb = c.sb("Ssb", [128, 16, 128], F32, a3); mxs = c.sb("mxs", [128, 16], F32, a3)
                    T16 = c.sb("T16", [128, 16, 16], F32, a3); tmpk = c.sb("tmpk", [128, 256], F32, a3)
                    Pc = c.sb("Pc", [128, 8, 256], F32, a3); c8 = c.sb("c8", [128, 8, 16], F32, a3)
                    zz = c.sb("zz", [128, 8], F32, a3)
                    tmpP = [c.sb("tmpP%d" % i, [128, 32, 128], F32, a3) for i in range(2)]; tmpPb = [c.sb("tmpPb%d" % i, [128, 32 * 128], BF16, a3) for i in range(2)]; Gcb = [c.sb("Gcb%d" % i, [128, 32 * 128], BF16, a3) for i in range(2)]
                    pss = [c.ps("pss%d" % i, [128, 512], F32, a3) for i in range(4)]
                    gn = 0
                    for i in range(4):
                        for ch in range(16):
                            c.op("pe", lambda e: e.matmul(pss[ch // 4][:, (ch % 4) * 128:(ch % 4 + 1) * 128], qpT[:, ch, i * 128:(i + 1) * 128], P["keysT"][:, ch, :], start=True, stop=True),
                                 r=["qpT", "keysT", "pss%d" % (ch // 4)], w=["pss%d" % (ch // 4)])
                        for q4 in range(4):
                            c.op("dve", lambda e: e.tensor_copy(Ssb[:, q4 * 4:(q4 + 1) * 4, :].rearrange("p a b -> p (a b)"), pss[q4][:]), r=["pss%d" % q4, "Ssb"], w=["Ssb"])
                        c.op("dve", lambda e: e.tensor_reduce(out=mxs[:], in_=Ssb[:], axis=AX.X, op=ALU.max), r=["Ssb"], w=["mxs"])
                        c.op("dve", lambda e: e.tensor_tensor(out=Ssb[:], in0=Ssb[:], in1=mxs[:].unsqueeze(2).to_broadcast([128, 16, 128]), op=ALU.subtract), r=["Ssb", "mxs"], w=["Ssb"])
                        c.op("act", lambda e: e.activation(out=Ssb[:].rearrange("p a b -> p (a b)"), in_=Ssb[:].rearrange("p a b -> p (a b)"), func=AF.Exp), r=["Ssb"], w=["Ssb"])
                        for ch in range(16):
                            c.op("dve", lambda e: e.max(out=T16[:, ch, 0:8], in_=Ssb[:, ch, :]), r=["Ssb", "T16"], w=["T16"])
                            c.op("dve", lambda e: e.match_replace(out=tmpk[:, 0:128], in_to_replace=T16[:, ch, 0:8], in_values=Ssb[:, ch, :], imm_value=-1.0), r=["Ssb", "T16"], w=["tmpk"])
                            c.op("dve", lambda e: e.max(out=T16[:, ch, 8:16], in_=tmpk[:, 0:128]), r=["tmpk", "T16"], w=["T16"])
                        T4 = T16[:].rearrange("p (h c) k -> p h c k", c=2)
                        c.op("dve", lambda e: e.tensor_tensor(out=Pc[:].rearrange("p h (a b) -> p h a b", a=16), in0=T4[:, :, 0, :].unsqueeze(3).to_broadcast([128, 8, 16, 16]),
                                                               in1=T4[:, :, 1, :].unsqueeze(2).to_broadcast([128, 8, 16, 16]), op=ALU.mult), r=["T16"], w=["Pc"])
                        for hh in range(8):
                            c.op("dve", lambda e: e.max(out=c8[:, hh, 0:8], in_=Pc[:, hh, :]), r=["Pc", "c8"], w=["c8"])
                            c.op("dve", lambda e: e.match_replace(out=tmpk[:], in_to_replace=c8[:, hh, 0:8], in_values=Pc[:, hh, :], imm_value=-1.0), r=["Pc", "c8"], w=["tmpk"])
                            c.op("dve", lambda e: e.max(out=c8[:, hh, 8:16], in_=tmpk[:]), r=["tmpk", "c8"], w=["c8"])
                        c.op("dve", lambda e: e.tensor_reduce(out=zz[:], in_=c8[:], axis=AX.X, op=ALU.add), r=["c8"], w=["zz"])
                        c.op("dve", lambda e: e.reciprocal(out=zz[:], in_=zz[:]), r=["zz"], w=["zz"])
                        for q in range(4):
                            gb = gn % 2; gn += 1
                            for hh in range(8):
                                tp = tmpP[hh % 2]; tk = "tmpP%d" % (hh % 2)
                                if hh % 2 == 0:
                                    c.op("pool", lambda e: e.tensor_tensor(out=tp[:], in0=Ssb[:, 2 * hh, q * 32:(q + 1) * 32].unsqueeze(2).to_broadcast([128, 32, 128]),
                                                                            in1=Ssb[:, 2 * hh + 1, :].unsqueeze(1).to_broadcast([128, 32, 128]), op=ALU.mult), r=["Ssb"], w=[tk])
                                else:
                                    for i1 in range(32):
                                        c.op("act", lambda e: e.activation(out=tp[:, i1, :], in_=Ssb[:, 2 * hh + 1, :], func=AF.Copy, scale=Ssb[:, 2 * hh, q * 32 + i1:q * 32 + i1 + 1]), r=["Ssb", tk], w=[tk])
                                tf = tp[:].rearrange("p a b -> p (a b)")
                                tb = tmpPb[hh % 2]; tbk = "tmpPb%d" % (hh % 2)
                                c.op("dve", lambda e: e.scalar_tensor_tensor(out=tb[:], in0=tf, scalar=c8[:, hh, 15:16], in1=tf, op0=ALU.is_ge, op1=ALU.mult), r=[tk, "c8"], w=[tbk])
                                if hh == 0:
                                    c.op("dve", lambda e: e.tensor_scalar(out=Gcb[gb][:], in0=tb[:], scalar1=zz[:, hh:hh + 1], scalar2=None, op0=ALU.mult), r=[tbk, "zz"], w=["Gcb%d" % gb])
                                else:
                                    c.op("dve", lambda e: e.scalar_tensor_tensor(out=Gcb[gb][:], in0=tb[:], scalar=zz[:, hh:hh + 1], in1=Gcb[gb][:], op0=ALU.mult, op1=ALU.add), r=[tbk, "zz", "Gcb%d" % gb], w=["Gcb%d" % gb])
                            r0 = ob * 512 + i * 128
                            c.dma("sp", S["G"][r0:r0 + 128, q * 4096:(q + 1) * 4096], Gcb[gb][:], r=["Gcb%d" % gb], w=["G_d"], stream="Gcb%d" % gb)
                    c.barrier()
            with contextlib.ExitStack() as bes:
                dn = [c.sb("dn%d" % i, [128, 16, 512], BF16, bes) for i in range(2)]
                upb = [c.sb("upb%d" % i, [128, 4, D], BF16, bes) for i in range(2)]
                gsb = [c.sb("gsb%d" % i, [128, 4, 512], BF16, bes) for i in range(2)]
                oacc = c.sb("oacc", [128, 4, D], F32, bes)
                gl = [c.sb("gl%d" % i, [128, 512], F32, bes) for i in range(2)]
                at = [c.sb("at%d" % i, [128, 512], BF16, bes) for i in range(2)]
                atT = [c.sb("atT%d" % i, [128, 4, 128], BF16, bes) for i in range(2)]
                hb = c.sb("hb", [128, D], F32, bes)
                ppre = [c.ps("ppre%d" % i, [128, 512], F32, bes) for i in range(2)]
                ptr = c.ps("ptrb", [128, 1024], BF16, bes)
                po = [c.ps("po%d" % i, [128, 512], F32, bes) for i in range(4)]
                dv = S["downT_bf"].rearrange("(c p) n -> p c n", p=128)
                NEB = NEXP // 512
                items = [(eb, i) for eb in range(NEB) for i in range(4)]
                def LOADW(eb):
                    wb = eb % 2
                    c.dma("sp", dn[wb][:], dv[:, :, eb * 512:(eb + 1) * 512], r=["downT_bf"], w=["dn%d" % wb], stream="dn%d" % wb)
                    c.dma("sp", upb[wb][:], S["up_bf"][eb * 512:(eb + 1) * 512, :].rearrange("(c p) n -> p c n", p=128), r=["up_bf"], w=["upb%d" % wb], stream="upb%d" % wb)
                    c.dma("pool", gsb[wb][:], S["G"][ob * 512:(ob + 1) * 512, eb * 512:(eb + 1) * 512].rearrange("(n p) c -> p n c", p=128), r=["G_d"], w=["gsb%d" % wb], stream="gsb%d" % wb)
                def PRE(n):
                    eb, i = items[n]; wb = eb % 2; bi = n % 2
                    if i == 0: LOADW(eb)
                    for kc in range(16):
                        c.op("pe", lambda e: e.matmul(ppre[bi][:], hn2T[:, kc, i * 128:(i + 1) * 128], dn[wb][:, kc, :], start=(kc == 0), stop=(kc == 15)), r=["hn2T", "dn%d" % wb], w=["ppre%d" % bi])
                def ACTS(n):
                    eb, i = items[n]; wb = eb % 2; bi = n % 2
                    c.op("act", lambda e: e.activation(out=gl[bi][:], in_=ppre[bi][:], func=AF.Gelu), r=["ppre%d" % bi], w=["gl%d" % bi])
                    c.op("dve", lambda e: e.tensor_tensor(out=at[bi][:], in0=gl[bi][:], in1=gsb[wb][:, i, :], op=ALU.mult), r=["gl%d" % bi, "gsb%d" % wb], w=["at%d" % bi])
                def TRUP(n):
                    eb, i = items[n]; wb = eb % 2; bi = n % 2
                    for ec in range(4):
                        c.op("pe", lambda e: e.transpose(ptr[:, bi * 512 + ec * 128:bi * 512 + (ec + 1) * 128], at[bi][:, ec * 128:(ec + 1) * 128], P["ident"][:]), r=["at%d" % bi, "ident"], w=["ptr%d" % bi])
                    c.op("act", lambda e: e.activation(out=atT[bi][:].rearrange("p a b -> p (a b)"), in_=ptr[:, bi * 512:(bi + 1) * 512], func=AF.Copy), r=["ptr%d" % bi], w=["atT%d" % bi])
                def UP(n):
                    eb, i = items[n]; wb = eb % 2; bi = n % 2
                    for db in range(4):
                        for ec in range(4):
                            c.op("pe", lambda e: e.matmul(po[db][:], atT[bi][:, ec, :], upb[wb][:, ec, db * 512:(db + 1) * 512], start=(ec == 0), stop=(ec == 3)), r=["atT%d" % bi, "upb%d" % wb], w=["po%d" % db])
                        ds_ = slice(db * 512, (db + 1) * 512)
                        if eb == 0:
                            c.op("dve", lambda e: e.tensor_copy(oacc[:, i, ds_], po[db][:]), r=["po%d" % db, "oacc%d" % i], w=["oacc%d" % i])
                        else:
                            c.op("dve", lambda e: e.tensor_tensor(out=oacc[:, i, ds_], in0=oacc[:, i, ds_], in1=po[db][:], op=ALU.add), r=["po%d" % db, "oacc%d" % i], w=["oacc%d" % i])
                PRE(0)
                for n in range(len(items)):
                    if n + 1 < len(items): PRE(n + 1)
                    ACTS(n)
                    TRUP(n)
                    if n >= 1: UP(n - 1)
                UP(len(items) - 1)
                for i in range(4):
                    r0 = ob * 512 + i * 128
                    c.dma("sp", hb[:], S["h"][r0:r0 + 128, :], r=["h_d"], w=["hb"], stream="hb")
                    c.op("dve", lambda e: e.tensor_tensor(out=oacc[:, i, :], in0=oacc[:, i, :], in1=P["g2b"][:], op=ALU.mult), r=["oacc%d" % i, "g2b"], w=["oacc%d" % i])
                    c.op("dve", lambda e: e.tensor_tensor(out=oacc[:, i, :], in0=oacc[:, i, :], in1=hb[:], op=ALU.add), r=["oacc%d" % i, "hb"], w=["oacc%d" % i])
                    c.dma("pool", y_out[r0:r0 + 128, :], oacc[:, i, :], r=["oacc%d" % i], w=["y"], stream="yout")
                c.barrier()
        c.barrier()


_NC_CACHE = {}

def kernel(**inputs):
    cfg = Cfg(int(np.asarray(inputs["x"]).shape[1]))
    if cfg.SEQ not in _NC_CACHE:
        _NC_CACHE[cfg.SEQ] = build(cfg)
    nc = _NC_CACHE[cfg.SEQ]
    maps = prep_inputs(inputs, cfg)
    res = run_bass_kernel_spmd(nc, maps, core_ids=list(range(8)))
    B = np.asarray(inputs["x"]).shape[0]
    out = np.zeros((B, cfg.SEQ, D), np.float32)
    for core in range(8):
        b, j = core // 4, core % 4
        out[b, j * cfg.QTR:(j + 1) * cfg.QTR] = np.asarray(res.results[core]["y"])
    return out
```
